# Optimizing a Trainium2 kernel written in Bass

```python
import jax, jax.numpy as jnp
from jax import lax
import numpy as np

D_MODEL = 1024
BATCH = 8
SEQ = 4096
DEPTH = 1

N_META = 16
BLOCK = 128
LEAD_PAD = BLOCK - N_META
SB_HEAD_DIM = 64
SB_WIDTH = D_MODEL // 2
SB_HEADS = SB_WIDTH // SB_HEAD_DIM
HG_HEAD_DIM = 128
HG_WIDTH = D_MODEL - SB_WIDTH
HG_HEADS = HG_WIDTH // HG_HEAD_DIM
MIX_WIDTH = SB_WIDTH + HG_WIDTH
IN_SPLITS = [SB_WIDTH, SB_WIDTH, SB_WIDTH, HG_WIDTH, HG_WIDTH, HG_WIDTH, HG_WIDTH]
IN_PROJ_WIDTH = sum(IN_SPLITS)
N_GROUPS = 4
EXPERTS_PER_GROUP = 8
N_EXPERTS = N_GROUPS * EXPERTS_PER_GROUP
TOP_K_IN_GROUP = 2
EXPERT_FF = D_MODEL // 2
MOE_BLOCK = 128
EPS = 1e-6

kernel_name = 'hymba_stickbreak_hgrn2_hier_moe'


def rmsnorm(x, gain):
    xf = x.astype(jnp.float32)
    y = xf * lax.rsqrt(jnp.mean(xf * xf, axis=-1, keepdims=True) + EPS)
    return (y * gain.astype(jnp.float32)).astype(x.dtype)


def split_heads(t, n_heads, head_dim):
    b, l, _ = t.shape
    return t.reshape(b, l, n_heads, head_dim).transpose(0, 2, 1, 3)


def merge_heads_norm(o, gain):
    b, h, l, d = o.shape
    o = rmsnorm(o.transpose(0, 2, 1, 3), gain.reshape(h, d))
    return o.reshape(b, l, h * d)


def stick_breaking_attention(q, k, v):
    L = q.shape[2]
    scale = SB_HEAD_DIM ** -0.5
    outs = []
    for blk in range(L // BLOCK):
        t0 = blk * BLOCK
        t1 = t0 + BLOCK
        z = jnp.einsum('bhtd,bhsd->bhts', q[:, :, t0:t1], k[:, :, :t1]).astype(jnp.float32) * scale
        t_pos = jnp.arange(t0, t1)[:, None]
        s_pos = jnp.arange(t1)[None, :]
        valid = (s_pos < t_pos) & (s_pos >= LEAD_PAD)
        log_beta = jax.nn.log_sigmoid(z)
        log_keep = jnp.where(valid, jax.nn.log_sigmoid(-z), 0.0)
        log_tail = lax.cumsum(log_keep, axis=3, reverse=True) - log_keep
        w = jnp.where(valid, jnp.exp(log_beta + log_tail), 0.0)
        outs.append(jnp.einsum('bhts,bhsd->bhtd', w.astype(v.dtype), v[:, :, :t1]))
    return jnp.concatenate(outs, axis=2)


def hgrn2_chunked(q, k, v, log_f):
    b, h, L, dk = q.shape
    dv = v.shape[-1]
    nc = L // BLOCK

    def to_chunks(t):
        return jnp.moveaxis(t.reshape(b, h, nc, BLOCK, t.shape[-1]), 2, 0)

    causal = jnp.tril(jnp.ones((BLOCK, BLOCK), bool))[:, :, None]

    def step(state, inp):
        qc, kc, vc, gc = inp
        bc = jnp.cumsum(gc, axis=2)
        diff = bc[:, :, :, None, :] - bc[:, :, None, :, :]
        decay = jnp.exp(jnp.where(causal, diff, -jnp.inf))
        scores = jnp.einsum('bhtd,bhsd,bhtsd->bhts', qc, kc, decay)
        o = jnp.einsum('bhts,bhsv->bhtv', scores, vc) + jnp.einsum('bhtd,bhdv->bhtv', qc * jnp.exp(bc), state)
        b_last = bc[:, :, -1, :]
        state = jnp.exp(b_last)[..., None] * state + jnp.einsum(
            'bhsd,bhsv->bhdv', kc * jnp.exp(b_last[:, :, None, :] - bc), vc)
        return state, o

    s0 = jnp.zeros((b, h, dk, dv), jnp.float32)
    _, o = lax.scan(step, s0, (to_chunks(q), to_chunks(k), to_chunks(v), to_chunks(log_f)))
    return jnp.moveaxis(o, 0, 2).reshape(b, h, L, dv)


def hierarchical_moe(x2, w_rg, b_rg, w_re, b_re, w_gate, w_up, w_down):
    n_tok = x2.shape[0]
    grp_probs = jax.nn.softmax(jnp.dot(x2, w_rg).astype(jnp.float32) + b_rg.astype(jnp.float32), axis=-1)
    grp = jnp.argmax(grp_probs, axis=-1)
    p_grp = jnp.max(grp_probs, axis=-1)
    exp_logits = (jnp.dot(x2, w_re).astype(jnp.float32) + b_re.astype(jnp.float32)).reshape(
        n_tok, N_GROUPS, EXPERTS_PER_GROUP)
    in_grp = exp_logits[jnp.arange(n_tok), grp]
    top_val, top_idx = lax.top_k(in_grp, TOP_K_IN_GROUP)
    gate = jax.nn.softmax(top_val, axis=-1) * p_grp[:, None]
    expert_id = (grp[:, None] * EXPERTS_PER_GROUP + top_idx).reshape(-1).astype(jnp.int32)
    gate_flat = gate.reshape(-1)
    token_id = jnp.repeat(jnp.arange(n_tok, dtype=jnp.int32), TOP_K_IN_GROUP)
    n_assign = n_tok * TOP_K_IN_GROUP
    n_slots = -(-n_assign // MOE_BLOCK) * MOE_BLOCK + N_EXPERTS * MOE_BLOCK
    n_blocks = n_slots // MOE_BLOCK
    counts = jnp.zeros((N_EXPERTS,), jnp.int32).at[expert_id].add(1)
    padded = (counts + MOE_BLOCK - 1) // MOE_BLOCK * MOE_BLOCK
    padded_end = jnp.cumsum(padded)
    padded_start = padded_end - padded
    start = jnp.cumsum(counts) - counts
    order = jnp.argsort(expert_id)
    sorted_e = expert_id[order]
    dest = padded_start[sorted_e] + jnp.arange(n_assign, dtype=jnp.int32) - start[sorted_e]
    slot_tok = jnp.zeros((n_slots,), jnp.int32).at[dest].set(token_id[order])
    slot_gate = jnp.zeros((n_slots,), jnp.float32).at[dest].set(gate_flat[order])
    block_e = jnp.clip(jnp.searchsorted(padded_end, jnp.arange(n_blocks, dtype=jnp.int32) * MOE_BLOCK,
                                        side='right'), 0, N_EXPERTS - 1)

    def run_block(args):
        tok, g, e = args
        xb = x2[tok]
        hb = jax.nn.silu(xb @ w_gate[e]) * (xb @ w_up[e])
        return (hb @ w_down[e]) * g[:, None].astype(x2.dtype)

    y_blocks = lax.map(run_block, (slot_tok.reshape(n_blocks, MOE_BLOCK),
                                   slot_gate.reshape(n_blocks, MOE_BLOCK), block_e))
    return jnp.zeros_like(x2).at[slot_tok].add(y_blocks.reshape(n_slots, -1))


def setup_inputs(seed: int = 0) -> dict:
    key = jax.random.key(seed)
    ks = jax.random.split(key, 18)
    f32 = jnp.float32

    def nrm(k, shape, scale):
        return jax.random.normal(k, shape, f32) * scale

    return {
        'x': nrm(ks[0], (BATCH, SEQ, D_MODEL), 1.0),
        'meta_tokens': nrm(ks[1], (N_META, D_MODEL), 1.0),
        'lb_logits': nrm(ks[2], (DEPTH + 1, HG_WIDTH), 0.1).at[-1].add(2.0),
        'g_mix': 1.0 + nrm(ks[3], (DEPTH, D_MODEL), 0.02),
        'w_in': nrm(ks[4], (DEPTH, D_MODEL, IN_PROJ_WIDTH), D_MODEL ** -0.5),
        'sb_gain': 1.0 + nrm(ks[5], (DEPTH, SB_WIDTH), 0.02),
        'hg_gain': 1.0 + nrm(ks[6], (DEPTH, HG_WIDTH), 0.02),
        'w_out': nrm(ks[7], (DEPTH, MIX_WIDTH, D_MODEL), MIX_WIDTH ** -0.5),
        'g_ffn': 1.0 + nrm(ks[8], (DEPTH, D_MODEL), 0.02),
        'w_router_group': nrm(ks[9], (DEPTH, D_MODEL, N_GROUPS), D_MODEL ** -0.5),
        'b_router_group': nrm(ks[10], (DEPTH, N_GROUPS), 0.01),
        'w_router_expert': nrm(ks[11], (DEPTH, D_MODEL, N_EXPERTS), D_MODEL ** -0.5),
        'b_router_expert': nrm(ks[12], (DEPTH, N_EXPERTS), 0.01),
        'w_expert_gate': nrm(ks[13], (DEPTH, N_EXPERTS, D_MODEL, EXPERT_FF), D_MODEL ** -0.5),
        'w_expert_up': nrm(ks[14], (DEPTH, N_EXPERTS, D_MODEL, EXPERT_FF), D_MODEL ** -0.5),
        'w_expert_down': nrm(ks[15], (DEPTH, N_EXPERTS, EXPERT_FF, D_MODEL), EXPERT_FF ** -0.5),
        'g_final': 1.0 + nrm(ks[16], (D_MODEL,), 0.02),
    }


def reference(x, meta_tokens, lb_logits, g_mix, w_in, sb_gain, hg_gain, w_out, g_ffn,
              w_router_group, b_router_group, w_router_expert, b_router_expert,
              w_expert_gate, w_expert_up, w_expert_down, g_final):
    b = x.shape[0]
    lead = jnp.zeros((b, LEAD_PAD, D_MODEL), x.dtype)
    meta = jnp.broadcast_to(meta_tokens.astype(x.dtype)[None], (b, N_META, D_MODEL))
    h = jnp.concatenate([lead, meta, x], axis=1)
    L = h.shape[1]
    real = (jnp.arange(L) >= LEAD_PAD)[None, :, None]
    lower_bounds = jnp.cumsum(jax.nn.softmax(lb_logits.astype(jnp.float32), axis=0), axis=0)
    split_idx = np.cumsum(IN_SPLITS)[:-1].tolist()

    for layer in range(DEPTH):
        a = rmsnorm(h, g_mix[layer])
        proj = jnp.einsum('bld,de->ble', a, w_in[layer])
        sb_q, sb_k, sb_v, hg_q, hg_f, hg_i, hg_g = jnp.split(proj, split_idx, axis=-1)

        o_sb = stick_breaking_attention(split_heads(sb_q, SB_HEADS, SB_HEAD_DIM),
                                        split_heads(sb_k, SB_HEADS, SB_HEAD_DIM),
                                        split_heads(sb_v, SB_HEADS, SB_HEAD_DIM))
        o_sb = merge_heads_norm(o_sb, sb_gain[layer]).astype(proj.dtype)

        lb = lower_bounds[layer]
        f_pre = hg_f.astype(jnp.float32)
        log_f = jnp.log(lb + (1.0 - lb) * jax.nn.sigmoid(f_pre))
        k_in = jnp.where(real, (1.0 - lb) * jax.nn.sigmoid(-f_pre), 0.0)
        q_in = jax.nn.silu(hg_q.astype(jnp.float32))
        o_hg = hgrn2_chunked(split_heads(q_in, HG_HEADS, HG_HEAD_DIM),
                             split_heads(k_in, HG_HEADS, HG_HEAD_DIM),
                             split_heads(hg_i.astype(jnp.float32), HG_HEADS, HG_HEAD_DIM),
                             split_heads(log_f, HG_HEADS, HG_HEAD_DIM))
        o_hg = (merge_heads_norm(o_hg, hg_gain[layer]) * jax.nn.silu(hg_g.astype(jnp.float32))).astype(proj.dtype)

        mixed = jnp.concatenate([o_sb, o_hg], axis=-1)
        h = h + jnp.einsum('ble,ed->bld', mixed, w_out[layer])

        m = rmsnorm(h[:, LEAD_PAD:], g_ffn[layer])
        n_pos = m.shape[1]
        y = hierarchical_moe(m.reshape(b * n_pos, D_MODEL), w_router_group[layer], b_router_group[layer],
                             w_router_expert[layer], b_router_expert[layer], w_expert_gate[layer],
                             w_expert_up[layer], w_expert_down[layer])
        h = h + jnp.pad(y.reshape(b, n_pos, D_MODEL), ((0, 0), (LEAD_PAD, 0), (0, 0)))

    out = rmsnorm(h[:, BLOCK:], g_final)
    return out
```

```python
import threading
import numpy as np
from contextlib import ExitStack
import concourse.bass as bass
import concourse.mybir as mybir
from concourse.bass_utils import run_bass_kernel_spmd

F32 = mybir.dt.float32
BF16 = mybir.dt.bfloat16
I32 = mybir.dt.int32
AF = mybir.ActivationFunctionType
ALU = mybir.AluOpType
AX = mybir.AxisListType

D = 1024
NE = 32
CAP = 384
NSLOT = NE * CAP
EPS = 1e-6
BIG = 1.0e4
CONV_EVERY = 6


_TLS = threading.local()


class Side:
    def __init__(self, fn):
        self.go = threading.Semaphore(0)
        self.ev = threading.Semaphore(0)
        self.finished = False
        self.err = None

        def run():
            self.go.acquire()
            _TLS.side = self
            try:
                fn()
            except BaseException as ex:
                self.err = ex
            self.finished = True
            self.ev.release()
        self.th = threading.Thread(target=run, daemon=True)
        self.th.start()

    def pause(self):
        self.ev.release()
        self.go.acquire()

    def step(self, n=1):
        for _ in range(n):
            if self.finished:
                break
            self.go.release()
            self.ev.acquire()
        if self.err is not None:
            raise self.err
        return not self.finished

    def drain(self):
        while self.step(64):
            pass


_AUTO = dict(sides=[], every=1, n=0)


def _pause():
    sd = getattr(_TLS, 'side', None)
    if sd is not None:
        sd.pause()
        return
    if _AUTO['sides']:
        _AUTO['n'] += 1
        if _AUTO['n'] % _AUTO['every'] == 0:
            for x in _AUTO['sides']:
                x.step(1)


def run_with_side(main_fn, side_fn, every):
    sd = Side(side_fn) if side_fn is not None else None
    _AUTO['sides'] = [sd] if sd is not None else []
    _AUTO['every'] = every
    try:
        main_fn()
    finally:
        _AUTO['sides'] = []
    if sd is not None:
        sd.drain()


class Prog:
    def __init__(self, nc, es):
        self.nc = nc
        self.es = es
        self.eng = dict(pe=nc.tensor, act=nc.scalar, dve=nc.vector, pool=nc.gpsimd, sp=nc.sync)
        self.sem = {k: es.enter_context(nc.semaphore("s_" + k)) for k in self.eng}
        self.cnt = {k: 0 for k in self.eng}
        self.seen = {k: {} for k in self.eng}
        self.lastw = {}
        self.readers = {}
        self.dsem = {}
        self.stream = {k: [] for k in self.eng}
        self.nsem = 0
        self.rr = 0

    def _deps(self, eng, reads, writes):
        deps = {}

        def add(d):
            if d is None:
                return
            k, v = d
            if k == 'pe' and eng == 'pe':
                return
            if deps.get(k, 0) < v:
                deps[k] = v

        for r in reads:
            add(self.lastw.get(r))
        for w in writes:
            add(self.lastw.get(w))
            for rd in self.readers.get(w, ()):
                add(rd)
        waits = []
        for k, v in deps.items():
            if self.seen[eng].get(k, 0) < v:
                self.seen[eng][k] = v
                waits.append((k, v))
        return waits

    def _commit(self, ident, reads, writes):
        for r in reads:
            self.readers.setdefault(r, []).append(ident)
        for w in writes:
            self.lastw[w] = ident
            self.readers[w] = []

    def _semof(self, k):
        if isinstance(k, str):
            return self.sem[k]
        return self.dsem[k[1]][0]

    def op(self, eng, fn, reads=(), writes=()):
        waits = self._deps(eng, reads, writes)
        self.cnt[eng] += 1
        ident = (eng, self.cnt[eng])
        self.stream[eng].append((waits, fn, [(eng, 1)]))
        self._commit(ident, reads, writes)
        _pause()
        return ident

    def dma(self, queue, res, fns, reads=(), writes=()):
        if res not in self.dsem:
            self.dsem[res] = [self.es.enter_context(self.nc.semaphore("d%d" % self.nsem)), 0]
            self.nsem += 1
        waits = self._deps(queue, reads, writes)
        self.dsem[res][1] += 16 * len(fns)
        ident = (('d', res), self.dsem[res][1])
        for i, fn in enumerate(fns):
            self.stream[queue].append((waits if i == 0 else [], fn, [(('d', res), 16)]))
        self._commit(ident, reads, writes)
        _pause()
        return ident

    def barrier(self):
        allid = [(k, v) for k, v in self.cnt.items() if v > 0]
        allid += [(('d', r), sv[1]) for r, sv in self.dsem.items() if sv[1] > 0]
        for eng in self.eng:
            self.final_wait(eng, allid)

    def final_wait(self, eng, idents):
        waits = []
        for k, v in idents:
            if k == eng:
                continue
            if self.seen[eng].get(k, 0) < v:
                self.seen[eng][k] = v
                waits.append((k, v))
        if waits:
            self.stream[eng].append((waits, None, []))

    def emit(self):
        nc = self.nc
        with nc.Block() as block:
            def run(name):
                def body(e):
                    for waits, fn, incs in self.stream[name]:
                        for k, v in waits:
                            e.wait_ge(self._semof(k), v)
                        if fn is None:
                            continue
                        ins = fn(e)
                        for k, v in incs:
                            ins.then_inc(self._semof(k), v)
                    self.stream[name] = []
                return body
            block.tensor(run('pe'))
            block.scalar(run('act'))
            block.vector(run('dve'))
            block.gpsimd(run('pool'))
            block.sync(run('sp'))


def seq(fns):
    def f(e):
        r = None
        for g in fns:
            r = g(e)
        return r
    return f


def MM(out, lhsT, rhs, start=True, stop=True, tp=None):
    if tp is None:
        return lambda e: e.matmul(out, lhsT=lhsT, rhs=rhs, start=start, stop=stop)
    return lambda e: e.matmul(out, lhsT=lhsT, rhs=rhs, start=start, stop=stop, tile_position=tp)


def TR(out, in_, ident):
    return lambda e: e.transpose(out, in_, ident)


def ACT(out, in_, func, **kw):
    return lambda e: e.activation(out=out, in_=in_, func=func, **kw)


def TT(out, in0, in1, op):
    return lambda e: e.tensor_tensor(out=out, in0=in0, in1=in1, op=op)


def TS(out, in0, s1, op0, s2=None, op1=None):
    if op1 is None:
        return lambda e: e.tensor_scalar(out=out, in0=in0, scalar1=s1, scalar2=None, op0=op0)
    return lambda e: e.tensor_scalar(out=out, in0=in0, scalar1=s1, scalar2=s2, op0=op0, op1=op1)


def STT(out, in0, scalar, in1, op0, op1):
    return lambda e: e.scalar_tensor_tensor(out=out, in0=in0, scalar=scalar, in1=in1, op0=op0, op1=op1)


def CP(out, in_):
    return lambda e: e.tensor_copy(out=out, in_=in_)


def DMA(out, in_):
    return lambda e: e.dma_start(out=out, in_=in_)


C_ID, C_UP, C_ONE, C_MD, C_TB, C_TR, C_SEL, C_RM, C_OFF, C_END = 0, 128, 256, 384, 512, 640, 768, 772, 773, 805


def make_consts():
    c = np.zeros((128, C_END), np.float32)
    i = np.arange(128)
    sub = i // 32
    c[:, C_ID:C_ID + 128] = np.eye(128)
    c[:, C_UP:C_UP + 128] = (i[:, None] >= i[None, :])
    c[:, C_ONE:C_ONE + 128] = 1.0
    c[:, C_MD:C_MD + 128] = (i[:, None] < i[None, :])
    same = sub[:, None] == sub[None, :]
    c[:, C_TB:C_TB + 128] = same & (i[:, None] <= i[None, :])
    c[:, C_TR:C_TR + 128] = same & (i[:, None] > i[None, :])
    c[:, C_SEL:C_SEL + 4] = sub[:, None] == np.arange(4)[None, :]
    c[:, C_RM] = (i >= 112)
    c[:, C_OFF:C_OFF + 32] = (np.arange(32) * CAP)[None, :]
    return c


def build(NPC=8, debug=False):
    NB = 1 + 4 * NPC
    L = NB * 128
    SEQ = 4 * NPC * 128
    nc = bass.Bass("TRN2", target_bir_lowering=False)
    din = lambda n, s, dt=F32: nc.dram_tensor(n, s, dt, kind="ExternalInput").ap()
    x = din("x", [SEQ, D])
    meta = din("meta_tokens", [16, D])
    lbl = din("lb_logits", [2, 512])
    g_mix = din("g_mix", [D])
    w_in = din("w_in", [D, 3584])
    sb_gain = din("sb_gain", [512])
    hg_gain = din("hg_gain", [512])
    w_out = din("w_out", [D, D])
    g_ffn = din("g_ffn", [D])
    w_rt = din("w_rt", [D, 36])
    b_rt = din("b_rt", [36])
    w_eg = din("w_expert_gate", [NE, D, 512])
    w_eu = din("w_expert_up", [NE, D, 512])
    w_ed = din("w_expert_down", [NE, 512, D])
    g_fin = din("g_final", [D])
    cst = din("consts", [128, C_END])
    out = nc.dram_tensor("out", [SEQ, D], F32, kind="ExternalOutput").ap()
    dscr = lambda n, s, dt: nc.dram_tensor(n, s, dt, kind=("ExternalOutput" if debug else "Internal")).ap()
    QTS = dscr("qts", [NPC, 128, 4 * 512], BF16)
    MHS = dscr("mhs", [NB, 128, 4 * 128], BF16)
    H1S = dscr("h1s", [NB, 128, D], F32)
    XS = dscr("xs", [NSLOT + 128, D], BF16)
    WGS = dscr("wgs", [NE, D, 512], BF16)
    WUS = dscr("wus", [NE, D, 512], BF16)
    WDS = dscr("wds", [NE, 512, D], BF16)
    YS = dscr("ys", [NSLOT + 128, D], BF16)

    with ExitStack() as es:
        P = Prog(nc, es)
        BCR = nc.gpsimd.alloc_register("bcreg")
        P.stream['pool'].append(([], lambda e: e.reg_mov(BCR, NSLOT - 1), []))
        T = lambda st, name, shape, dt: st.enter_context(nc.sbuf_tensor(name, shape, dt))
        PALL = es.enter_context(nc.psum_tensor("pall", [128, 4096], F32))
        pb = [PALL[:, i * 512:(i + 1) * 512] for i in range(8)]

        BANKS = [[2, 3, 4, 5, 6, 7]]

        def bank():
            i = BANKS[0][P.rr % len(BANKS[0])]
            P.rr += 1
            return pb[i], 'pb%d' % i

        C32 = T(es, "c32", [128, C_END], F32)
        C16 = T(es, "c16", [128, 512], BF16)
        G1 = T(es, "g1", [128, NB], F32)
        G2 = T(es, "g2", [128, NB], F32)
        DS1 = T(es, "ds1", [128, NB], I32)
        DS2 = T(es, "ds2", [128, NB], I32)
        P.dma('sp', 'c32', [DMA(C32[:], cst)], writes=['c32'])
        P.op('dve', CP(C16[:], C32[:, 0:512]), reads=['c32'], writes=['c16'])
        ID16 = C16[:, C_ID:C_ID + 128]
        UP16 = C16[:, C_UP:C_UP + 128]
        ONE16 = C16[:, C_ONE:C_ONE + 128]
        MD16 = C16[:, C_MD:C_MD + 128]
        TB32 = C32[:, C_TB:C_TB + 128]
        TR32 = C32[:, C_TR:C_TR + 128]
        SEL32 = C32[:, C_SEL:C_SEL + 4]
        RM32 = C32[:, C_RM:C_RM + 1]
        OFF32 = C32[:, C_OFF:C_OFF + 32]

        s12 = ExitStack()
        KT = T(s12, "kt", [128, 4, L], BF16)
        V = T(s12, "v", [128, NB, 512], BF16)
        with ExitStack() as s1:
            WIN = T(s1, "win", [128, 8, 3584], BF16)
            GMIX = T(s1, "gmix", [128, D], F32)
            OML = T(s1, "oml", [128, 512], F32)
            HGG = T(s1, "hgg", [128, 512], F32)
            XB = [T(s1, "xb%d" % i, [128, D], F32) for i in range(2)]
            JK = T(s1, "jk", [128, D], BF16)
            A16 = [T(s1, "a16%d" % i, [128, D], BF16) for i in range(2)]
            ATS = [T(s1, "at%d" % i, [128, 8, 512], BF16) for i in range(2)]
            QTP = T(s1, "qtp", [128, 4, 512], BF16)
            ST1 = T(s1, "st1", [128, 8], F32)
            QS = T(s1, "qs", [128, 512], F32)
            SG = T(s1, "sg", [128, 512], F32)
            KU = T(s1, "ku", [128, 512], F32)
            LF = T(s1, "lf", [128, 512], F32)
            EB = T(s1, "eb", [128, 512], F32)
            EMB = T(s1, "emb", [128, 512], F32)
            ERC = T(s1, "erc", [128, 512], F32)
            GG = T(s1, "gg", [128, 512], F32)
            DEC = T(s1, "dec", [128, 16], F32)
            QT16 = T(s1, "qt16", [128, 512], BF16)
            KT16 = T(s1, "kt16", [128, 512], BF16)
            KH16 = T(s1, "kh16", [128, 512], BF16)
            VH16 = T(s1, "vh16", [128, 512], BF16)
            QTT = T(s1, "qtt", [128, 4, 128], BF16)
            KTT = T(s1, "ktt", [128, 4, 128], BF16)
            SCM = T(s1, "scm", [128, 4, 128], BF16)
            S32 = T(s1, "s32", [128, 4, 128], F32)
            S16 = T(s1, "s16", [128, 4, 4, 128], BF16)
            SSQ = T(s1, "ssq", [128, 4], F32)
            MH = T(s1, "mh", [128, 512], BF16)
            MHT = T(s1, "mht", [128, 512], BF16)

            for k in range(8):
                P.dma('pool', 'win', [DMA(WIN[:, k, :], w_in[k * 128:(k + 1) * 128, :])], writes=[('win', k)])
            WINR = [('win', k) for k in range(8)]
            P.dma('sp', 'gmix', [DMA(GMIX[:], g_mix.partition_broadcast(128))], writes=['gmix'])
            P.dma('sp', 'hgg', [DMA(HGG[:], hg_gain.partition_broadcast(128))], writes=['hgg'])
            P.dma('sp', 'lb0', [DMA(QS[:], lbl[0].partition_broadcast(128))], writes=['qs'])
            P.dma('sp', 'oml', [DMA(OML[:], lbl[1].partition_broadcast(128))], writes=['oml'])
            P.op('dve', TT(OML[:], OML[:], QS[:], ALU.subtract), reads=['oml', 'qs'], writes=['oml'])
            P.op('act', ACT(OML[:], OML[:], AF.Sigmoid), reads=['oml'], writes=['oml'])
            P.op('dve', lambda e: e.memset(S32[:], 0.0), writes=['s32'])
            P.op('dve', lambda e: e.memset(S16[:], 0.0), writes=[('s16', j) for j in range(4)])

            def load_norm_T(blk, col, xb_i, pc):
                AT = ATS[pc % 2]
                xb = XB[xb_i]
                a16 = A16[xb_i]
                xk = ('xb', xb_i)
                ak = ('a16', xb_i)
                if blk == 0:
                    P.op('dve', lambda e: e.memset(xb[:], 0.0), writes=[xk])
                    P.dma('sp', xk, [DMA(xb[112:128, :], meta)], reads=[], writes=[xk])
                else:
                    P.dma('sp', xk, [DMA(xb[:], x[(blk - 1) * 128:blk * 128, :])], writes=[xk])
                P.op('act', ACT(JK[:], xb[:], AF.Square, accum_out=ST1[:, 0:1]), reads=[xk], writes=['jk', 'st1a'])
                P.op('dve', TS(ST1[:, 1:2], ST1[:, 0:1], 1.0 / D, ALU.mult, EPS, ALU.add), reads=['st1a'], writes=['st1b'])
                P.op('act', ACT(ST1[:, 2:3], ST1[:, 1:2], AF.Ln), reads=['st1b'], writes=['st1c'])
                P.op('act', ACT(ST1[:, 3:4], ST1[:, 2:3], AF.Exp, scale=-0.5), reads=['st1c'], writes=['st1d'])
                P.op('dve', STT(a16[:], xb[:], ST1[:, 3:4], GMIX[:], ALU.mult, ALU.mult), reads=[xk, 'st1d', 'gmix'], writes=[ak])
                bk, bn = bank()
                bk16 = bk[:].bitcast(BF16)
                P.op('pe', seq([TR(bk16[:, k * 128:(k + 1) * 128], a16[:, k * 128:(k + 1) * 128], ID16) for k in range(8)]),
                     reads=[ak, 'c16'], writes=[bn])
                P.op('dve', CP(AT[:, :, col * 128:(col + 1) * 128], bk16[:].rearrange("p (k t) -> p k t", k=8)),
                     reads=[bn], writes=[('at', pc % 2, col)])

            def hgrn_block2(blk, col, pc):
                AT = ATS[pc % 2]
                atr = [('at', pc % 2, col)]
                asl = lambda k: AT[:, k, col * 128:(col + 1) * 128]

                def proj(c0):
                    bk, bn = bank()
                    P.op('pe', seq([MM(bk[:], asl(k), WIN[:, k, c0:c0 + 512], start=(k == 0), stop=(k == 7)) for k in range(8)]),
                         reads=atr + WINR, writes=[bn])
                    return bk, bn
                bk, bn = proj(1024)
                P.op('act', ACT(V[:, blk, :], bk[:], AF.Copy), reads=[bn], writes=[('v', blk)])
                bk, bn = proj(1536)
                P.op('act', ACT(QS[:], bk[:], AF.Sigmoid), reads=[bn], writes=['qs'])
                P.op('dve', TT(QS[:], QS[:], bk[:], ALU.mult), reads=['qs', bn], writes=['qs'])
                bk, bn = proj(2048)
                P.op('act', ACT(SG[:], bk[:], AF.Sigmoid, scale=-1.0), reads=[bn], writes=['sg'])
                bk, bn = proj(2560)
                P.op('act', ACT(VH16[:], bk[:], AF.Copy), reads=[bn], writes=['vh16'])
                bk, bn = proj(3072)
                P.op('act', ACT(GG[:], bk[:], AF.Sigmoid), reads=[bn], writes=['gg'])
                P.op('dve', TT(GG[:], GG[:], bk[:], ALU.mult), reads=['gg', bn], writes=['gg'])
                P.op('dve', TT(GG[:], GG[:], HGG[:], ALU.mult), reads=['gg', 'hgg'], writes=['gg'])
                P.op('dve', TT(KU[:], SG[:], OML[:], ALU.mult), reads=['sg', 'oml'], writes=['ku'])
                P.op('act', ACT(LF[:], KU[:], AF.Ln, scale=-1.0, bias=1.0), reads=['ku'], writes=['lf'])
                if blk == 0:
                    P.op('dve', TS(KU[:], KU[:], RM32, ALU.mult), reads=['ku', 'c32', 'lf'], writes=['ku'])
                km, kmk = KU, 'ku'
                bbc, bbcn = bank()
                P.op('pe', MM(bbc[:], TB32, LF[:]), reads=['lf', 'c32'], writes=[bbcn])
                brc, brcn = bank()
                P.op('pe', MM(brc[:], TR32, LF[:]), reads=['lf', 'c32'], writes=[brcn])
                bdl, bdln = bank()
                P.op('pe', seq([MM(bdl[:, h * 4:(h + 1) * 4], LF[:, h * 128:(h + 1) * 128], SEL32) for h in range(4)]),
                     reads=['lf', 'c32'], writes=[bdln])
                P.op('act', ACT(EB[:], bbc[:], AF.Exp), reads=[bbcn], writes=['eb'])
                P.op('act', ACT(EMB[:], bbc[:], AF.Exp, scale=-1.0), reads=[bbcn], writes=['emb'])
                P.op('act', ACT(ERC[:], brc[:], AF.Exp), reads=[brcn], writes=['erc'])
                P.op('act', ACT(DEC[:], bdl[:, 0:16], AF.Exp), reads=[bdln], writes=['dec'])
                P.op('dve', TT(QT16[:], QS[:], EB[:], ALU.mult), reads=['qs', 'eb'], writes=['qt16'])
                P.op('dve', TT(KT16[:], km[:], EMB[:], ALU.mult), reads=[kmk, 'emb'], writes=['kt16'])
                P.op('dve', TT(KH16[:], km[:], ERC[:], ALU.mult), reads=[kmk, 'erc'], writes=['kh16'])
                if blk > 0:
                    bq, bqn = bank()
                    bq16 = bq[:].bitcast(BF16)
                    P.op('pe', seq([TR(bq16[:, h * 128:(h + 1) * 128], QT16[:, h * 128:(h + 1) * 128], ID16) for h in range(4)] +
                                   [TR(bq16[:, 512 + h * 128:512 + (h + 1) * 128], KT16[:, h * 128:(h + 1) * 128], ID16) for h in range(4)]),
                         reads=['qt16', 'kt16', 'c16'], writes=[bqn])
                    P.op('dve', CP(QTT[:].rearrange("p h t -> p (h t)"), bq16[:, 0:512]), reads=[bqn], writes=['qtt'])
                    P.op('dve', CP(KTT[:].rearrange("p h t -> p (h t)"), bq16[:, 512:1024]), reads=[bqn], writes=['ktt'])
                    bs, bsn = bank()
                    P.op('pe', seq([MM(bs[:, h * 128:(h + 1) * 128], KTT[:, h, :], QTT[:, h, :]) for h in range(4)]),
                         reads=['qtt', 'ktt'], writes=[bsn])
                    P.op('dve', TT(SCM[:], bs[:].rearrange("p (h t) -> p h t", h=4),
                                   TB32.unsqueeze(1).to_broadcast([128, 4, 128]), ALU.mult), reads=[bsn, 'c32'], writes=['scm'])
                    bo, bon = pb[blk % 2], 'pb%d' % (blk % 2)
                    P.op('pe', seq([MM(bo[:, h * 128:(h + 1) * 128], SCM[:, h, :], VH16[:, h * 128:(h + 1) * 128], start=(h == 0), stop=False)
                                    for h in range(4)]), reads=['scm', 'vh16'], writes=[bon])
                dec3 = DEC[:].rearrange("p (h j) -> p h j", h=4)
                for j in range(4):
                    if blk > 0:
                        P.op('pe', seq([MM(bo[32 * j:32 * j + 32, h * 128:(h + 1) * 128], QTT[:, h, 32 * j:32 * j + 32], S16[:, j, h, :],
                                           start=False, stop=(j == 3), tp=(0, 32 * j)) for h in range(4)]),
                             reads=['qtt', ('s16', j)], writes=[bon])
                    bu, bun = bank()
                    P.op('pe', seq([MM(bu[:, h * 128:(h + 1) * 128], KH16[32 * j:32 * j + 32, h * 128:(h + 1) * 128],
                                       VH16[32 * j:32 * j + 32, h * 128:(h + 1) * 128], tp=(32 * j, 0)) for h in range(4)]),
                         reads=['kh16', 'vh16'], writes=[bun])
                    P.op('dve', TT(S32[:], S32[:], dec3[:, :, j:j + 1].to_broadcast([128, 4, 128]), ALU.mult),
                         reads=['s32', 'dec'], writes=['s32'])
                    P.op('dve', TT(S32[:].rearrange("p h v -> p (h v)"), S32[:].rearrange("p h v -> p (h v)"), bu[:], ALU.add),
                         reads=['s32', bun], writes=['s32'])
                    jn = (j + 1) % 4
                    P.op('dve', CP(S16[:, jn, :, :], S32[:]), reads=['s32'], writes=[('s16', jn)])
                if blk == 0:
                    return
                P.op('act', ACT(EB[:], bo[:], AF.Square), reads=[bon], writes=['eb'])
                P.op('dve', lambda e: e.tensor_reduce(out=SSQ[:], in_=EB[:].rearrange("p (h v) -> p h v", h=4), axis=AX.X, op=ALU.add),
                     reads=['eb'], writes=['ssq'])
                P.op('dve', TS(SSQ[:], SSQ[:], 1.0 / 128, ALU.mult, EPS, ALU.add), reads=['ssq'], writes=['ssq'])
                P.op('act', ACT(SSQ[:], SSQ[:], AF.Ln), reads=['ssq'], writes=['ssq'])
                P.op('act', ACT(SSQ[:], SSQ[:], AF.Exp, scale=-0.5), reads=['ssq'], writes=['ssq'])
                for h in range(4):
                    P.op('dve', STT(MH[:, h * 128:(h + 1) * 128], bo[:, h * 128:(h + 1) * 128], SSQ[:, h:h + 1],
                                    GG[:, h * 128:(h + 1) * 128], ALU.mult, ALU.mult), reads=[bon, 'ssq', 'gg'], writes=[('mh', h)])
                bt, btn = bank()
                bt16 = bt[:].bitcast(BF16)
                P.op('pe', seq([TR(bt16[:, h * 128:(h + 1) * 128], MH[:, h * 128:(h + 1) * 128], ID16) for h in range(4)]),
                     reads=[('mh', h) for h in range(4)] + ['c16'], writes=[btn])
                P.op('dve', CP(MHT[:], bt16[:, 0:512]), reads=[btn], writes=['mht'])
                P.dma('sp', 'mhs', [DMA(MHS[blk], MHT[:])], reads=['mht'], writes=[('mhs', blk)])

            def front1(pc):
                blks = [0] if pc == 0 else list(range(4 * (pc - 1) + 1, 4 * pc + 1))
                AT = ATS[pc % 2]
                nt = 128 * len(blks)
                for c, blk in enumerate(blks):
                    load_norm_T(blk, c, c % 2, pc)
                atr = [('at', pc % 2, c) for c in range(len(blks))]
                for et in range(8):
                    if pc == 0 and et < 4:
                        continue
                    bk, bn = bank()
                    P.op('pe', seq([MM(bk[:, 0:nt], WIN[:, k, et * 128:(et + 1) * 128], AT[:, k, 0:nt], start=(k == 0), stop=(k == 7))
                                    for k in range(8)]), reads=atr + WINR, writes=[bn])
                    if et < 4:
                        P.op('act', ACT(QTP[:, et, :], bk[:], AF.Copy), reads=[bn], writes=[('qtp', et)])
                    else:
                        t0 = blks[0] * 128
                        P.op('act', ACT(KT[:, et - 4, t0:t0 + nt], bk[:, 0:nt], AF.Copy), reads=[bn], writes=[('kt', pc)])
                if pc >= 1:
                    P.dma('sp', 'qts', [DMA(QTS[pc - 1], QTP[:].rearrange("p e t -> p (e t)"))],
                          reads=[('qtp', et) for et in range(4)], writes=[('qts', pc)])

            def back1(pc):
                blks = [0] if pc == 0 else list(range(4 * (pc - 1) + 1, 4 * pc + 1))
                for c, blk in enumerate(blks):
                    hgrn_block2(blk, c, pc)

            front1(0)
            for pc in range(NPC + 1):
                run_with_side(lambda pc=pc: back1(pc), (lambda pc=pc: front1(pc + 1)) if pc < NPC else None, 3)
            P.barrier()
            P.emit()

        with ExitStack() as s2:
            BANKS[0] = [4, 5]
            WOUT = T(s2, "wout", [128, 8, D], BF16)
            WR = T(s2, "wr", [128, 8, 36], BF16)
            BR = T(s2, "br", [128, 36], F32)
            SBG = T(s2, "sbg", [128, 512], F32)
            GFFN = T(s2, "gffn", [128, D], F32)
            QTB = [T(s2, "qt%d" % i, [128, 4, 512], BF16) for i in range(2)]
            QTNB = [T(s2, "qtn%d" % i, [128, 4, 512], BF16) for i in range(2)]
            E32 = [T(s2, "e32%d" % i, [128, 2, 512], F32) for i in range(2)]
            SP16 = [T(s2, "sp16%d" % i, [128, 2, 512], BF16) for i in range(4)]
            W16 = [T(s2, "w16%d" % i, [128, 2, 512], BF16) for i in range(3)]
            SACC = [T(s2, "sacc%d" % i, [128, 2, 512], BF16) for i in range(2)]
            OSB = [T(s2, "osb%d" % i, [128, 4, 512], F32) for i in range(2)]
            SQ = T(s2, "sq", [128, 512], F32)
            ST2 = T(s2, "st2", [128, 8], F32)
            ST3 = T(s2, "st3", [128, 8], F32)
            MX = T(s2, "mx", [128, 512], F32)
            MX16 = T(s2, "mx16", [128, 512], BF16)
            MXT = T(s2, "mxt", [128, 8, 128], BF16)
            XR = T(s2, "xr", [128, D], F32)
            H1 = T(s2, "h1", [128, D], F32)
            JK2 = T(s2, "jk2", [128, D], F32)
            M16 = [T(s2, "m16%d" % i, [128, D], BF16) for i in range(2)]
            MT = T(s2, "mt", [128, 8, 128], BF16)
            LG = T(s2, "lg", [128, 36], F32)
            RT = T(s2, "rt", [128, 16], F32)
            GE = T(s2, "ge", [128, 4], F32)
            GSEL = T(s2, "gsel", [128, 4], F32)
            ML = T(s2, "ml", [128, 32], F32)
            ML2 = T(s2, "ml2", [128, 32], F32)
            M1 = T(s2, "m1", [128, 32], F32)
            M2 = T(s2, "m2", [128, 32], F32)
            M12 = T(s2, "m12", [128, 32], BF16)
            CNT = T(s2, "cnt", [128, 32], F32)
            DST = T(s2, "dst", [128, 32], F32)
            DJ = T(s2, "dj", [128, 32], F32)

            ZT = T(s2, "zt", [128, D], BF16)
            def load_qt(pc):
                P.dma('sp', ('qt', pc % 2), [DMA(QTB[pc % 2][:].rearrange("p e t -> p (e t)"), QTS[pc - 1])],
                      reads=[('qts', pc)], writes=[('qt', pc % 2)])
            for pc_ in range(1, min(NPC, 2) + 1):
                load_qt(pc_)
            P.op('pool', lambda e: e.memset(ZT[:], 0.0), reads=[('qt', 0), ('qt', 1)], writes=['zt'])
            conv = [f for ex in range(NE) for f in ((WGS, w_eg, ex), (WUS, w_eu, ex), (WDS, w_ed, ex))]
            conv_i = [0]
            stepc = [0]

            def conv_next():
                if conv_i[0] < len(conv):
                    dst, src, ex = conv[conv_i[0]]
                    conv_i[0] += 1
                    P.dma('pool', 'wcv', [DMA(dst[ex], src[ex])], writes=[('wcv', ex, conv_i[0] % 3)])
            xs_z = XS[0:NSLOT, :].rearrange("(n p) d -> n p d", p=128)
            for n in range(NSLOT // 128):
                P.dma('pool', 'xsz', [DMA(xs_z[n], ZT[:])], reads=['zt'], writes=['xs'])
            for k in range(8):
                P.dma('pool', 'wout', [DMA(WOUT[:, k, :], w_out[k * 128:(k + 1) * 128, :])], writes=[('wout', k)])
            P.dma('pool', 'wr', [DMA(WR[:], w_rt.rearrange("(k p) e -> p k e", p=128))], writes=['wr'])
            P.dma('sp', 'br', [DMA(BR[:], b_rt.partition_broadcast(128))], writes=['br'])
            P.dma('sp', 'sbg', [DMA(SBG[:], sb_gain.partition_broadcast(128))], writes=['sbg'])
            P.dma('sp', 'gffn', [DMA(GFFN[:], g_ffn.partition_broadcast(128))], writes=['gffn'])
            P.op('dve', lambda e: e.memset(CNT[:], 0.0), writes=['cnt'])
            WOUTR = [('wout', k) for k in range(8)]
            tcount = [0]

            def sb_piece(pc, sides=()):
                b0 = 4 * (pc - 1) + 1
                osb = OSB[pc % 2]
                QT, QTN = QTB[pc % 2], QTNB[pc % 2]
                qk, qnk = ('qt', pc % 2), ('qtn', pc % 2)
                if 2 <= pc < NPC:
                    load_qt(pc + 1)
                P.op('dve', TS(QTN[:], QT[:], -0.125, ALU.mult), reads=[qk], writes=[qnk])
                kb_first = b0 + 3
                zsel = [0]
                for g in range(2):
                    for i in range(2):
                        P.op('dve', lambda e, i=i: e.memset(SACC[i][:], 0.0), writes=[('sacc', i)])
                    for bi in range(2):
                        P.op('dve', lambda e, bi=bi: e.memset(pb[bi][:], 0.0), writes=['pb%d' % bi])
                    tiles = []
                    for kb in range(kb_first, -1, -1):
                        for i in range(2):
                            t = 2 * g + i
                            j = kb - b0
                            c0 = 128 * j if j > 0 else 0
                            tiles.append(dict(i=i, t=t, kb=kb, j=j, c0=c0, n=512 - c0))
                    nt = len(tiles)

                    zkeys = ['pb2', 'pb3']
                    z2 = PALL[:, 2 * 512:4 * 512].rearrange("p (u c) -> p u c", u=2)

                    def stageA_pe(T_):
                        n, c0, t, kb = T_['n'], T_['c0'], T_['t'], T_['kb']
                        P.op('pe', seq([MM(z2[:, u, 0:n], KT[64 * u:64 * u + 64, t, kb * 128:(kb + 1) * 128],
                                           QT[64 * u:64 * u + 64, t, c0:512], tp=(64 * u, 0)) for u in range(2)]),
                             reads=[qk, ('kt', (kb + 3) // 4)], writes=zkeys)

                    def stageA(T_):
                        k = tcount[0]
                        tcount[0] += 1
                        T_['e'], T_['s'], T_['w'] = k % 2, k % 4, k % 3
                        n, c0, t, kb = T_['n'], T_['c0'], T_['t'], T_['kb']
                        e32, sp16 = E32[T_['e']], SP16[T_['s']]
                        ek, sk = ('e32', T_['e']), ('sp16', T_['s'])
                        P.op('act', ACT(e32[:, :, 0:n], z2[:, :, 0:n], AF.Exp, scale=0.125), reads=zkeys, writes=[ek])
                        P.op('act', ACT(sp16[:, :, 0:n], e32[:, :, 0:n], AF.Ln, bias=1.0), reads=[ek], writes=[sk])
                        if T_['j'] >= 0:
                            P.op('dve', TT(sp16[:, :, 0:128], sp16[:, :, 0:128], MD16.unsqueeze(1).to_broadcast([128, 2, 128]), ALU.mult),
                                 reads=[sk, 'c16'], writes=[sk])
                        if kb == 0:
                            P.op('dve', TS(sp16[:, :, 0:n], sp16[:, :, 0:n], RM32, ALU.mult), reads=[sk, 'c32'], writes=[sk])

                    def stageB(T_):
                        n, c0, t, kb, i = T_['n'], T_['c0'], T_['t'], T_['kb'], T_['i']
                        sp16, w16 = SP16[T_['s']], W16[T_['w']]
                        sk, wk, sak = ('sp16', T_['s']), ('w16', T_['w']), ('sacc', i)
                        t2 = PALL[:, 6 * 512:8 * 512].rearrange("p (u c) -> p u c", u=2)
                        mms = []
                        for u in range(2):
                            mms.append(MM(t2[:, u, 0:n], UP16, sp16[:, u, 0:n], start=True, stop=False))
                            if kb != kb_first:
                                mms.append(MM(t2[:, u, 0:n], ONE16, SACC[i][:, u, c0:512], start=False, stop=False))
                        for u in range(2):
                            kts = KT[64 * u:64 * u + 64, t, kb * 128:(kb + 1) * 128]
                            mms.append(MM(t2[:, u, 0:n], kts, QTN[64 * u:64 * u + 64, t, c0:512], start=False, stop=True, tp=(64 * u, 0)))
                        P.op('pe', seq(mms), reads=[sk, sak, qnk, 'c16', ('kt', (kb + 3) // 4)], writes=['pb6', 'pb7'])
                        P.op('act', ACT(w16[:, :, 0:n], t2[:, :, 0:n], AF.Exp, scale=-1.0), reads=['pb6', 'pb7'], writes=[wk])
                        if T_['j'] >= 0:
                            P.op('dve', TT(w16[:, :, 0:128], w16[:, :, 0:128], MD16.unsqueeze(1).to_broadcast([128, 2, 128]), ALU.mult),
                                 reads=[wk, 'c16'], writes=[wk])
                        if kb == 0:
                            P.op('dve', TS(w16[:, :, 0:n], w16[:, :, 0:n], RM32, ALU.mult), reads=[wk, 'c32'], writes=[wk])

                    def stageC(T_):
                        n, c0, kb, i, t, j = T_['n'], T_['c0'], T_['kb'], T_['i'], T_['t'], T_['j']
                        sp16, w16 = SP16[T_['s']], W16[T_['w']]
                        sk, wk, sak = ('sp16', T_['s']), ('w16', T_['w']), ('sacc', i)
                        if kb > 0:
                            P.op('dve', TT(SACC[i][:, :, c0:512], SACC[i][:, :, c0:512], sp16[:, :, 0:n], ALU.add), reads=[sak, sk], writes=[sak])
                        q0 = j if j > 0 else 0
                        ob, obn = pb[i], 'pb%d' % i
                        mms = []
                        for u in range(2):
                            h = 2 * t + u
                            for q in range(q0, 4):
                                mms.append(MM(ob[:, u * 256 + q * 64:u * 256 + (q + 1) * 64], w16[:, u, (q - q0) * 128:(q - q0 + 1) * 128],
                                              V[:, kb, h * 64:(h + 1) * 64], start=False, stop=(kb == 0)))
                        P.op('pe', seq(mms), reads=[wk, ('v', kb)], writes=[obn])

                    stageA_pe(tiles[0])
                    for step in range(nt + 2):
                        if step < nt:
                            stageA(tiles[step])
                        if 0 <= step - 1 < nt:
                            stageB(tiles[step - 1])
                        if 0 <= step - 2 < nt:
                            stageC(tiles[step - 2])
                        if step + 1 < nt:
                            stageA_pe(tiles[step + 1])
                        stepc[0] += 1
                        if stepc[0] % CONV_EVERY == 0:
                            conv_next()
                        for sd in sides:
                            sd.step(6)
                    for i in range(2):
                        t = 2 * g + i
                        P.op('dve', CP(osb[:, :, t * 128:(t + 1) * 128].rearrange("p q (u d) -> p q u d", u=2),
                                       pb[i][:, :].rearrange("p (u q d) -> p q u d", u=2, q=4)),
                             reads=['pb%d' % i], writes=[('osb', pc % 2, 2 * t), ('osb', pc % 2, 2 * t + 1)])

            def rstd_chain(buf, n, scale, kbase):
                P.op('dve', TS(buf[:, 0:n], buf[:, 0:n], scale, ALU.mult, EPS, ALU.add), reads=[kbase], writes=[kbase])
                P.op('act', ACT(buf[:, 0:n], buf[:, 0:n], AF.Ln), reads=[kbase], writes=[kbase])
                P.op('act', ACT(buf[:, 0:n], buf[:, 0:n], AF.Exp, scale=-0.5), reads=[kbase], writes=[kbase])

            def post_block(pc, q):
                blk = 4 * (pc - 1) + 1 + q
                osr = [('osb', pc % 2, h) for h in range(8)]
                oq = OSB[pc % 2][:, q, :]
                P.dma('sp', 'mxtb', [DMA(MXT[:, 4:8, :].rearrange("p k t -> p (k t)"), MHS[blk])], reads=[('mhs', blk)], writes=['mxtb'])
                P.dma('sp', 'xr', [DMA(XR[:], x[(blk - 1) * 128:blk * 128, :])], writes=['xr'])
                P.op('dve', TT(SQ[:], oq, oq, ALU.mult), reads=osr, writes=['sq'])
                P.op('dve', lambda e: e.tensor_reduce(out=ST2[:], in_=SQ[:].rearrange("p (h d) -> p h d", h=8), axis=AX.X, op=ALU.add),
                     reads=['sq'], writes=['st2'])
                rstd_chain(ST2, 8, 1.0 / 64, 'st2')
                P.op('dve', TT(MX[:].rearrange("p (h d) -> p h d", h=8), oq.rearrange("p (h d) -> p h d", h=8),
                               ST2[:].unsqueeze(2).to_broadcast([128, 8, 64]), ALU.mult), reads=osr + ['st2'], writes=['mx'])
                P.op('dve', TT(MX16[:], MX[:], SBG[:], ALU.mult), reads=['mx', 'sbg'], writes=['mx16'])
                bt, btn = bank()
                bt16 = bt[:].bitcast(BF16)
                P.op('pe', seq([TR(bt16[:, k * 128:(k + 1) * 128], MX16[:, k * 128:(k + 1) * 128], ID16) for k in range(4)]),
                     reads=['mx16', 'c16'], writes=[btn])
                P.op('dve', CP(MXT[:, 0:4, :].rearrange("p k t -> p (k t)"), bt16[:, 0:512]), reads=[btn], writes=['mxta'])
                for hf in range(2):
                    bo, bon = bank()
                    P.op('pe', seq([MM(bo[:], MXT[:, k, :], WOUT[:, k, hf * 512:(hf + 1) * 512], start=(k == 0), stop=(k == 7)) for k in range(8)]),
                         reads=['mxta', 'mxtb'] + WOUTR, writes=[bon])
                    P.op('dve', TT(H1[:, hf * 512:(hf + 1) * 512], bo[:], XR[:, hf * 512:(hf + 1) * 512], ALU.add),
                         reads=[bon, 'xr'], writes=[('h1', hf)])
                h1r = [('h1', 0), ('h1', 1)]
                P.dma('sp', 'h1s', [DMA(H1S[blk], H1[:])], reads=h1r, writes=[('h1s', blk)])
                P.op('act', ACT(JK2[:], H1[:], AF.Square, accum_out=ST3[:, 0:1]), reads=h1r, writes=['jk2', 'st3'])
                rstd_chain(ST3, 1, 1.0 / D, 'st3')
                mi = blk % 2
                m16, mk = M16[mi], ('m16', mi)
                P.op('dve', STT(m16[:], H1[:], ST3[:, 0:1], GFFN[:], ALU.mult, ALU.mult), reads=h1r + ['st3', 'gffn'], writes=[mk])
                bt, btn = bank()
                bt16 = bt[:].bitcast(BF16)
                P.op('pe', seq([TR(bt16[:, k * 128:(k + 1) * 128], m16[:, k * 128:(k + 1) * 128], ID16) for k in range(8)]),
                     reads=[mk, 'c16'], writes=[btn])
                P.op('dve', CP(MT[:].rearrange("p k t -> p (k t)"), bt16[:]), reads=[btn], writes=['mt'])
                bl, bln = bank()
                P.op('pe', seq([MM(bl[:, 0:36], MT[:, k, :], WR[:, k, :], start=(k == 0), stop=(k == 7)) for k in range(8)]),
                     reads=['mt', 'wr'], writes=[bln])
                P.op('dve', TT(LG[:], bl[:, 0:36], BR[:], ALU.add), reads=[bln, 'br'], writes=['lg'])
                r = lambda i: RT[:, i:i + 1]
                P.op('dve', lambda e: e.reduce_max(out=r(0), in_=LG[:, 0:4], axis=AX.X), reads=['lg'], writes=['r0'])
                P.op('dve', TS(r(1), r(0), -1.0, ALU.mult), reads=['r0'], writes=['r1'])
                P.op('act', ACT(GE[:], LG[:, 0:4], AF.Exp, bias=r(1), accum_out=r(2)), reads=['lg', 'r1'], writes=['ge', 'r2'])
                P.op('dve', lambda e: e.reciprocal(out=r(3), in_=r(2)), reads=['r2'], writes=['r3'])
                P.op('dve', TS(GSEL[:], LG[:, 0:4], r(0), ALU.is_equal), reads=['lg', 'r0'], writes=['gsel'])
                P.op('dve', TS(GSEL[:], GSEL[:], 1.0, ALU.subtract, BIG, ALU.mult), reads=['gsel'], writes=['gsel'])
                P.op('dve', TT(ML[:].rearrange("p (g j) -> p g j", g=4), LG[:, 4:36].rearrange("p (g j) -> p g j", g=4),
                               GSEL[:].unsqueeze(2).to_broadcast([128, 4, 8]), ALU.add), reads=['lg', 'gsel'], writes=['ml'])
                P.op('dve', lambda e: e.reduce_max(out=r(4), in_=ML[:], axis=AX.X), reads=['ml'], writes=['r4'])
                P.op('dve', TS(M1[:], ML[:], r(4), ALU.is_equal), reads=['ml', 'r4'], writes=['m1'])
                P.op('dve', STT(ML2[:], M1[:], -BIG, ML[:], ALU.mult, ALU.add), reads=['m1', 'ml'], writes=['ml2'])
                P.op('dve', lambda e: e.reduce_max(out=r(5), in_=ML2[:], axis=AX.X), reads=['ml2'], writes=['r5'])
                P.op('dve', TS(M2[:], ML2[:], r(5), ALU.is_equal), reads=['ml2', 'r5'], writes=['m2'])
                P.op('dve', TT(r(6), r(5), r(4), ALU.subtract), reads=['r4', 'r5'], writes=['r6'])
                P.op('act', ACT(r(7), r(6), AF.Exp), reads=['r6'], writes=['r7'])
                P.op('dve', TS(r(8), r(7), 1.0, ALU.add), reads=['r7'], writes=['r8'])
                P.op('dve', lambda e: e.reciprocal(out=r(9), in_=r(8)), reads=['r8'], writes=['r9'])
                P.op('dve', TT(G1[:, blk:blk + 1], r(9), r(3), ALU.mult), reads=['r9', 'r3'], writes=[('g1', blk)])
                P.op('dve', TT(G2[:, blk:blk + 1], r(3), G1[:, blk:blk + 1], ALU.subtract), reads=['r3', ('g1', blk)], writes=[('g2', blk)])
                P.op('dve', TT(M12[:], M1[:], M2[:], ALU.add), reads=['m1', 'm2'], writes=['m12'])
                bc_, bcn = bank()
                P.op('pe', seq([MM(bc_[:, 0:32], MD16, M12[:]), MM(bc_[:, 32:64], ONE16, M12[:])]), reads=['m12', 'c16'], writes=[bcn])
                P.op('dve', TT(DST[:], bc_[:, 0:32], CNT[:], ALU.add), reads=[bcn, 'cnt'], writes=['dst'])
                P.op('dve', TT(CNT[:], CNT[:], bc_[:, 32:64], ALU.add), reads=[bcn, 'cnt', 'dst'], writes=['cnt'])
                P.op('dve', TS(DST[:], DST[:], float(CAP - 1), ALU.min), reads=['dst'], writes=['dst'])
                P.op('dve', TT(DST[:], DST[:], OFF32, ALU.add), reads=['dst', 'c32'], writes=['dst'])
                for (mm_, ds, nm) in ((M1, DS1, 'ds1'), (M2, DS2, 'ds2')):
                    P.op('dve', TT(DJ[:], DST[:], mm_[:], ALU.mult), reads=['dst', 'm1', 'm2'], writes=['dj'])
                    P.op('dve', lambda e: e.tensor_reduce(out=r(10), in_=DJ[:], axis=AX.X, op=ALU.add), reads=['dj'], writes=['r10'])
                    P.op('dve', CP(ds[:, blk:blk + 1], r(10)), reads=['r10'], writes=[(nm, blk)])
                    P.dma('pool', ('xs', nm), [lambda e, ds=ds, m16=m16: e.indirect_dma_start(
                        out=XS, out_offset=bass.IndirectOffsetOnAxis(ap=ds[:, blk:blk + 1], axis=0), in_=m16[:], in_offset=None,
                        bounds_check=BCR, oob_is_err=False)], reads=[(nm, blk), mk, 'xs'], writes=[('xsw', nm, blk)])

            def post_piece(pc):
                for q in range(4):
                    post_block(pc, q)

            for pc in range(1, NPC + 1):
                sides = [Side(lambda pc=pc: post_piece(pc - 1))] if pc > 1 else []
                sb_piece(pc, sides)
                for sd in sides:
                    sd.drain()
            while conv_i[0] < len(conv):
                conv_next()
            post_piece(NPC)
            P.barrier()
            P.emit()
        s12.close()

        with ExitStack() as s3:
            BANKS[0] = [2, 3, 4, 5, 6, 7]
            WG = [T(s3, "wg%d" % i, [128, 8, 512], BF16) for i in range(2)]
            WU = [T(s3, "wu%d" % i, [128, 8, 512], BF16) for i in range(2)]
            WD = [T(s3, "wd%d" % i, [128, 4, D], BF16) for i in range(2)]
            XG = [T(s3, "xg%d" % i, [128, 3, D], BF16) for i in range(2)]
            XTS = [T(s3, "xt%d" % i, [128, 8, CAP], BF16) for i in range(2)]
            SL = T(s3, "sl", [128, CAP], F32)
            HB = T(s3, "hb", [128, 4, CAP], BF16)
            YT = [T(s3, "yt%d" % i, [128, D], BF16) for i in range(2)]
            GFIN = T(s3, "gfin", [128, D], F32)
            Y1 = [T(s3, "y1%d" % i, [128, D], BF16) for i in range(4)]
            Y2 = [T(s3, "y2%d" % i, [128, D], BF16) for i in range(4)]
            HF = [T(s3, "hf%d" % i, [128, D], F32) for i in range(4)]
            JK3 = T(s3, "jk3", [128, D], F32)
            ST4 = T(s3, "st4", [128, 4], F32)
            P.dma('sp', 'gfin', [DMA(GFIN[:], g_fin.partition_broadcast(128))], writes=['gfin'])
            ycount = [0]

            def front3(ex):
                i2 = ex % 2
                wg, wu, wd, xg, xt = WG[i2], WU[i2], WD[i2], XG[i2], XTS[i2]
                P.dma('sp', ('xg', i2), [DMA(xg[:], XS[ex * CAP:(ex + 1) * CAP, :].rearrange("(s p) d -> p s d", p=128))], writes=[('xg', i2)])
                P.dma('sp', ('wg', i2), [DMA(wg[:], WGS[ex].rearrange("(k p) f -> p k f", p=128))], reads=[('wcv', ex, c_) for c_ in range(3)], writes=[('wg', i2)])
                P.dma('sp', ('wu', i2), [DMA(wu[:], WUS[ex].rearrange("(k p) f -> p k f", p=128))], reads=[('wcv', ex, c_) for c_ in range(3)], writes=[('wu', i2)])
                P.dma('pool', ('wd', i2), [DMA(wd[:], WDS[ex].rearrange("(k p) f -> p k f", p=128))], reads=[('wcv', ex, c_) for c_ in range(3)], writes=[('wd', i2)])
                for s_ in range(3):
                    bt, btn = bank()
                    bt16 = bt[:].bitcast(BF16)
                    P.op('pe', seq([TR(bt16[:, k * 128:(k + 1) * 128], xg[:, s_, k * 128:(k + 1) * 128], ID16) for k in range(8)]),
                         reads=[('xg', i2), 'c16'], writes=[btn])
                    src = bt16[:].rearrange("p (k t) -> p k t", k=8)
                    dst = xt[:, :, s_ * 128:(s_ + 1) * 128]
                    if s_ == 1:
                        P.op('act', (lambda e, dst=dst, src=src: e.copy(out=dst, in_=src)), reads=[btn], writes=[('xt', i2, s_)])
                    else:
                        P.op('dve', CP(dst, src), reads=[btn], writes=[('xt', i2, s_)])

            def back3(ex):
                i2 = ex % 2
                wg, wu, wd, xt = WG[i2], WU[i2], WD[i2], XTS[i2]
                xtr = [('xt', i2, s_) for s_ in range(3)]
                for ft in range(4):
                    bg, bgn = bank()
                    P.op('pe', seq([MM(bg[:, 0:CAP], wg[:, k, ft * 128:(ft + 1) * 128], xt[:, k, :], start=(k == 0), stop=(k == 7)) for k in range(8)]),
                         reads=xtr + [('wg', i2)], writes=[bgn])
                    bu, bun = bank()
                    P.op('pe', seq([MM(bu[:, 0:CAP], wu[:, k, ft * 128:(ft + 1) * 128], xt[:, k, :], start=(k == 0), stop=(k == 7)) for k in range(8)]),
                         reads=xtr + [('wu', i2)], writes=[bun])
                    P.op('act', ACT(SL[:], bg[:, 0:CAP], AF.Silu), reads=[bgn], writes=['sl'])
                    P.op('dve', TT(HB[:, ft, :], SL[:], bu[:, 0:CAP], ALU.mult), reads=['sl', bun], writes=[('hb', ft)])
                hbr = [('hb', ft) for ft in range(4)]
                for s_ in range(3):
                    yi = ycount[0] % 2
                    ycount[0] += 1
                    yt, yk = YT[yi], ('yt', yi)
                    for hf in range(2):
                        by, byn = bank()
                        P.op('pe', seq([MM(by[:], HB[:, ft, s_ * 128:(s_ + 1) * 128], wd[:, ft, hf * 512:(hf + 1) * 512], start=(ft == 0), stop=(ft == 3))
                                        for ft in range(4)]), reads=hbr + [('wd', i2)], writes=[byn])
                        if hf == 0:
                            P.op('act', ACT(yt[:, 0:512], by[:], AF.Copy), reads=[byn], writes=[(yk, 0)])
                        else:
                            P.op('dve', CP(yt[:, 512:1024], by[:]), reads=[byn], writes=[(yk, 1)])
                    r0 = ex * CAP + s_ * 128
                    P.dma('sp', ('ys', yi), [DMA(YS[r0:r0 + 128, :], yt[:])], reads=[(yk, 0), (yk, 1)], writes=[('ys', ex, s_)])

            front3(0)
            for ex in range(NE):
                run_with_side(lambda ex=ex: back3(ex), (lambda ex=ex: front3(ex + 1)) if ex + 1 < NE else None, 3)
            ysall = [('ys', ex, s) for ex in range(NE) for s in range(3)]
            stores = []
            for blk in range(1, NB):
                i2 = blk % 4
                y1, y2, hf_ = Y1[i2], Y2[i2], HF[i2]
                P.dma('pool', ('y1', i2), [lambda e, y1=y1, blk=blk: e.indirect_dma_start(
                    out=y1[:], out_offset=None, in_=YS, in_offset=bass.IndirectOffsetOnAxis(ap=DS1[:, blk:blk + 1], axis=0),
                    bounds_check=BCR, oob_is_err=False)], reads=ysall + [('ds1', blk)], writes=[('y1', i2)])
                P.dma('pool', ('y2', i2), [lambda e, y2=y2, blk=blk: e.indirect_dma_start(
                    out=y2[:], out_offset=None, in_=YS, in_offset=bass.IndirectOffsetOnAxis(ap=DS2[:, blk:blk + 1], axis=0),
                    bounds_check=BCR, oob_is_err=False)], reads=ysall + [('ds2', blk)], writes=[('y2', i2)])
                P.dma('sp', ('hf', i2), [DMA(hf_[:], H1S[blk])], reads=[('h1s', blk)], writes=[('hf', i2)])
                P.op('dve', STT(hf_[:], y1[:], G1[:, blk:blk + 1], hf_[:], ALU.mult, ALU.add), reads=[('y1', i2), ('g1', blk), ('hf', i2)], writes=[('hf', i2)])
                P.op('dve', STT(hf_[:], y2[:], G2[:, blk:blk + 1], hf_[:], ALU.mult, ALU.add), reads=[('y2', i2), ('g2', blk), ('hf', i2)], writes=[('hf', i2)])
                P.op('act', ACT(JK3[:], hf_[:], AF.Square, accum_out=ST4[:, 0:1]), reads=[('hf', i2)], writes=['jk3', 'st4'])
                rstd_chain(ST4, 1, 1.0 / D, 'st4')
                P.op('dve', STT(hf_[:], hf_[:], ST4[:, 0:1], GFIN[:], ALU.mult, ALU.mult), reads=[('hf', i2), 'st4', 'gfin'], writes=[('hf', i2)])
                stores.append(P.dma('sp', ('out', i2), [DMA(out[(blk - 1) * 128:blk * 128, :], hf_[:])], reads=[('hf', i2)], writes=[('out', blk)]))
            P.final_wait('sp', stores)
            P.emit()
    return nc


_CACHE = {}


def kernel(x, meta_tokens, lb_logits, g_mix, w_in, sb_gain, hg_gain, w_out, g_ffn, w_router_group, b_router_group,
           w_router_expert, b_router_expert, w_expert_gate, w_expert_up, w_expert_down, g_final, _npc=8, _cores=8, _debug=False):
    f = lambda a: np.ascontiguousarray(np.asarray(a), dtype=np.float32)
    x = f(x)
    if (_npc, _debug) not in _CACHE:
        _CACHE[(_npc, _debug)] = build(_npc, _debug)
    nc = _CACHE[(_npc, _debug)]
    shared = dict(
        meta_tokens=f(meta_tokens), lb_logits=f(lb_logits), g_mix=f(g_mix)[0], w_in=f(w_in)[0], sb_gain=f(sb_gain)[0],
        hg_gain=f(hg_gain)[0], w_out=f(w_out)[0], g_ffn=f(g_ffn)[0],
        w_rt=np.ascontiguousarray(np.concatenate([f(w_router_group)[0], f(w_router_expert)[0]], axis=1)),
        b_rt=np.ascontiguousarray(np.concatenate([f(b_router_group)[0], f(b_router_expert)[0]], axis=0)),
        w_expert_gate=f(w_expert_gate)[0], w_expert_up=f(w_expert_up)[0], w_expert_down=f(w_expert_down)[0],
        g_final=f(g_final), consts=make_consts())
    in_maps = [dict(shared, x=np.ascontiguousarray(x[c])) for c in range(_cores)]
    res = run_bass_kernel_spmd(nc, in_maps, core_ids=list(range(_cores)))
    if _debug:
        return res.results
    return np.stack([np.asarray(r["out"], dtype=np.float32) for r in res.results], axis=0)
```

```python
import threading
import numpy as np
from contextlib import ExitStack
import concourse.bass as bass
import concourse.mybir as mybir
from concourse.bass_utils import run_bass_kernel_spmd

F32 = mybir.dt.float32
BF16 = mybir.dt.bfloat16
I32 = mybir.dt.int32
AF = mybir.ActivationFunctionType
ALU = mybir.AluOpType
AX = mybir.AxisListType

D = 1024
NE = 32
CAP = 384
NSLOT = NE * CAP
EPS = 1e-6
BIG = 1.0e4
CONV_EVERY = 6


_TLS = threading.local()


class Side:
    def __init__(self, fn):
        self.go = threading.Semaphore(0)
        self.ev = threading.Semaphore(0)
        self.finished = False
        self.err = None

        def run():
            self.go.acquire()
            _TLS.side = self
            try:
                fn()
            except BaseException as ex:
                self.err = ex
            self.finished = True
            self.ev.release()
        self.th = threading.Thread(target=run, daemon=True)
        self.th.start()

    def pause(self):
        self.ev.release()
        self.go.acquire()

    def step(self, n=1):
        for _ in range(n):
            if self.finished:
                break
            self.go.release()
            self.ev.acquire()
        if self.err is not None:
            raise self.err
        return not self.finished

    def drain(self):
        while self.step(64):
            pass


_AUTO = dict(sides=[], every=1, n=0)


def _pause():
    sd = getattr(_TLS, 'side', None)
    if sd is not None:
        sd.pause()
        return
    if _AUTO['sides']:
        _AUTO['n'] += 1
        if _AUTO['n'] % _AUTO['every'] == 0:
            for x in _AUTO['sides']:
                x.step(1)


def run_with_side(main_fn, side_fn, every):
    sd = Side(side_fn) if side_fn is not None else None
    _AUTO['sides'] = [sd] if sd is not None else []
    _AUTO['every'] = every
    try:
        main_fn()
    finally:
        _AUTO['sides'] = []
    if sd is not None:
        sd.drain()


class Prog:
    def __init__(self, nc, es):
        self.nc = nc
        self.es = es
        self.eng = dict(pe=nc.tensor, act=nc.scalar, dve=nc.vector, pool=nc.gpsimd, sp=nc.sync)
        self.sem = {k: es.enter_context(nc.semaphore("s_" + k)) for k in self.eng}
        self.cnt = {k: 0 for k in self.eng}
        self.seen = {k: {} for k in self.eng}
        self.lastw = {}
        self.readers = {}
        self.dsem = {}
        self.stream = {k: [] for k in self.eng}
        self.nsem = 0
        self.rr = 0

    def _deps(self, eng, reads, writes):
        deps = {}

        def add(d):
            if d is None:
                return
            k, v = d
            if k == 'pe' and eng == 'pe':
                return
            if deps.get(k, 0) < v:
                deps[k] = v

        for r in reads:
            add(self.lastw.get(r))
        for w in writes:
            add(self.lastw.get(w))
            for rd in self.readers.get(w, ()):
                add(rd)
        waits = []
        for k, v in deps.items():
            if self.seen[eng].get(k, 0) < v:
                self.seen[eng][k] = v
                waits.append((k, v))
        return waits

    def _commit(self, ident, reads, writes):
        for r in reads:
            self.readers.setdefault(r, []).append(ident)
        for w in writes:
            self.lastw[w] = ident
            self.readers[w] = []

    def _semof(self, k):
        if isinstance(k, str):
            return self.sem[k]
        return self.dsem[k[1]][0]

    def op(self, eng, fn, reads=(), writes=()):
        waits = self._deps(eng, reads, writes)
        self.cnt[eng] += 1
        ident = (eng, self.cnt[eng])
        self.stream[eng].append((waits, fn, [(eng, 1)]))
        self._commit(ident, reads, writes)
        _pause()
        return ident

    def dma(self, queue, res, fns, reads=(), writes=()):
        if res not in self.dsem:
            self.dsem[res] = [self.es.enter_context(self.nc.semaphore("d%d" % self.nsem)), 0]
            self.nsem += 1
        waits = self._deps(queue, reads, writes)
        self.dsem[res][1] += 16 * len(fns)
        ident = (('d', res), self.dsem[res][1])
        for i, fn in enumerate(fns):
            self.stream[queue].append((waits if i == 0 else [], fn, [(('d', res), 16)]))
        self._commit(ident, reads, writes)
        _pause()
        return ident

    def barrier(self):
        allid = [(k, v) for k, v in self.cnt.items() if v > 0]
        allid += [(('d', r), sv[1]) for r, sv in self.dsem.items() if sv[1] > 0]
        for eng in self.eng:
            self.final_wait(eng, allid)

    def final_wait(self, eng, idents):
        waits = []
        for k, v in idents:
            if k == eng:
                continue
            if self.seen[eng].get(k, 0) < v:
                self.seen[eng][k] = v
                waits.append((k, v))
        if waits:
            self.stream[eng].append((waits, None, []))

    def emit(self):
        nc = self.nc
        with nc.Block() as block:
            def run(name):
                def body(e):
                    for waits, fn, incs in self.stream[name]:
                        for k, v in waits:
                            e.wait_ge(self._semof(k), v)
                        if fn is None:
                            continue
                        ins = fn(e)
                        for k, v in incs:
                            ins.then_inc(self._semof(k), v)
                    self.stream[name] = []
                return body
            block.tensor(run('pe'))
            block.scalar(run('act'))
            block.vector(run('dve'))
            block.gpsimd(run('pool'))
            block.sync(run('sp'))


def seq(fns):
    def f(e):
        r = None
        for g in fns:
            r = g(e)
        return r
    return f


def MM(out, lhsT, rhs, start=True, stop=True, tp=None):
    if tp is None:
        return lambda e: e.matmul(out, lhsT=lhsT, rhs=rhs, start=start, stop=stop)
    return lambda e: e.matmul(out, lhsT=lhsT, rhs=rhs, start=start, stop=stop, tile_position=tp)


def TR(out, in_, ident):
    return lambda e: e.transpose(out, in_, ident)


def ACT(out, in_, func, **kw):
    return lambda e: e.activation(out=out, in_=in_, func=func, **kw)


def TT(out, in0, in1, op):
    return lambda e: e.tensor_tensor(out=out, in0=in0, in1=in1, op=op)


def TS(out, in0, s1, op0, s2=None, op1=None):
    if op1 is None:
        return lambda e: e.tensor_scalar(out=out, in0=in0, scalar1=s1, scalar2=None, op0=op0)
    return lambda e: e.tensor_scalar(out=out, in0=in0, scalar1=s1, scalar2=s2, op0=op0, op1=op1)


def STT(out, in0, scalar, in1, op0, op1):
    return lambda e: e.scalar_tensor_tensor(out=out, in0=in0, scalar=scalar, in1=in1, op0=op0, op1=op1)


def CP(out, in_):
    return lambda e: e.tensor_copy(out=out, in_=in_)


def DMA(out, in_):
    return lambda e: e.dma_start(out=out, in_=in_)


C_ID, C_UP, C_ONE, C_MD, C_TB, C_TR, C_SEL, C_RM, C_OFF, C_END = 0, 128, 256, 384, 512, 640, 768, 772, 773, 805


def make_consts():
    c = np.zeros((128, C_END), np.float32)
    i = np.arange(128)
    sub = i // 32
    c[:, C_ID:C_ID + 128] = np.eye(128)
    c[:, C_UP:C_UP + 128] = (i[:, None] >= i[None, :])
    c[:, C_ONE:C_ONE + 128] = 1.0
    c[:, C_MD:C_MD + 128] = (i[:, None] < i[None, :])
    same = sub[:, None] == sub[None, :]
    c[:, C_TB:C_TB + 128] = same & (i[:, None] <= i[None, :])
    c[:, C_TR:C_TR + 128] = same & (i[:, None] > i[None, :])
    c[:, C_SEL:C_SEL + 4] = sub[:, None] == np.arange(4)[None, :]
    c[:, C_RM] = (i >= 112)
    c[:, C_OFF:C_OFF + 32] = (np.arange(32) * CAP)[None, :]
    return c


def build(NPC=8, debug=False):
    NB = 1 + 4 * NPC
    L = NB * 128
    SEQ = 4 * NPC * 128
    nc = bass.Bass("TRN2", target_bir_lowering=False)
    din = lambda n, s, dt=F32: nc.dram_tensor(n, s, dt, kind="ExternalInput").ap()
    x = din("x", [SEQ, D])
    meta = din("meta_tokens", [16, D])
    lbl = din("lb_logits", [2, 512])
    g_mix = din("g_mix", [D])
    w_in = din("w_in", [D, 3584])
    sb_gain = din("sb_gain", [512])
    hg_gain = din("hg_gain", [512])
    w_out = din("w_out", [D, D])
    g_ffn = din("g_ffn", [D])
    w_rt = din("w_rt", [D, 36])
    b_rt = din("b_rt", [36])
    w_eg = din("w_expert_gate", [NE, D, 512])
    w_eu = din("w_expert_up", [NE, D, 512])
    w_ed = din("w_expert_down", [NE, 512, D])
    g_fin = din("g_final", [D])
    cst = din("consts", [128, C_END])
    out = nc.dram_tensor("out", [SEQ, D], F32, kind="ExternalOutput").ap()
    dscr = lambda n, s, dt: nc.dram_tensor(n, s, dt, kind=("ExternalOutput" if debug else "Internal")).ap()
    QTS = dscr("qts", [NPC, 128, 4 * 512], BF16)
    MHS = dscr("mhs", [NB, 128, 4 * 128], BF16)
    H1S = dscr("h1s", [NB, 128, D], F32)
    XS = dscr("xs", [NSLOT + 128, D], BF16)
    WGS = dscr("wgs", [NE, D, 512], BF16)
    WUS = dscr("wus", [NE, D, 512], BF16)
    WDS = dscr("wds", [NE, 512, D], BF16)
    YS = dscr("ys", [NSLOT + 128, D], BF16)

    with ExitStack() as es:
        P = Prog(nc, es)
        BCR = nc.gpsimd.alloc_register("bcreg")
        P.stream['pool'].append(([], lambda e: e.reg_mov(BCR, NSLOT - 1), []))
        T = lambda st, name, shape, dt: st.enter_context(nc.sbuf_tensor(name, shape, dt))
        PALL = es.enter_context(nc.psum_tensor("pall", [128, 4096], F32))
        pb = [PALL[:, i * 512:(i + 1) * 512] for i in range(8)]

        BANKS = [[2, 3, 4, 5, 6, 7]]

        def bank():
            i = BANKS[0][P.rr % len(BANKS[0])]
            P.rr += 1
            return pb[i], 'pb%d' % i

        C32 = T(es, "c32", [128, C_END], F32)
        C16 = T(es, "c16", [128, 512], BF16)
        G1 = T(es, "g1", [128, NB], F32)
        G2 = T(es, "g2", [128, NB], F32)
        DS1 = T(es, "ds1", [128, NB], I32)
        DS2 = T(es, "ds2", [128, NB], I32)
        P.dma('sp', 'c32', [DMA(C32[:], cst)], writes=['c32'])
        P.op('dve', CP(C16[:], C32[:, 0:512]), reads=['c32'], writes=['c16'])
        ID16 = C16[:, C_ID:C_ID + 128]
        UP16 = C16[:, C_UP:C_UP + 128]
        ONE16 = C16[:, C_ONE:C_ONE + 128]
        MD16 = C16[:, C_MD:C_MD + 128]
        TB32 = C32[:, C_TB:C_TB + 128]
        TR32 = C32[:, C_TR:C_TR + 128]
        SEL32 = C32[:, C_SEL:C_SEL + 4]
        RM32 = C32[:, C_RM:C_RM + 1]
        OFF32 = C32[:, C_OFF:C_OFF + 32]

        s12 = ExitStack()
        KT = T(s12, "kt", [128, 4, L], BF16)
        V = T(s12, "v", [128, NB, 512], BF16)
        with ExitStack() as s1:
            WIN = T(s1, "win", [128, 8, 3584], BF16)
            GMIX = T(s1, "gmix", [128, D], F32)
            OML = T(s1, "oml", [128, 512], F32)
            HGG = T(s1, "hgg", [128, 512], F32)
            XB = [T(s1, "xb%d" % i, [128, D], F32) for i in range(2)]
            JK = T(s1, "jk", [128, D], BF16)
            A16 = [T(s1, "a16%d" % i, [128, D], BF16) for i in range(2)]
            ATS = [T(s1, "at%d" % i, [128, 8, 512], BF16) for i in range(2)]
            QTP = T(s1, "qtp", [128, 4, 512], BF16)
            ST1 = T(s1, "st1", [128, 8], F32)
            QS = T(s1, "qs", [128, 512], F32)
            SG = T(s1, "sg", [128, 512], F32)
            KU = T(s1, "ku", [128, 512], F32)
            LF = T(s1, "lf", [128, 512], F32)
            EB = T(s1, "eb", [128, 512], F32)
            EMB = T(s1, "emb", [128, 512], F32)
            ERC = T(s1, "erc", [128, 512], F32)
            GG = T(s1, "gg", [128, 512], F32)
            DEC = T(s1, "dec", [128, 16], F32)
            QT16 = T(s1, "qt16", [128, 512], BF16)
            KT16 = T(s1, "kt16", [128, 512], BF16)
            KH16 = T(s1, "kh16", [128, 512], BF16)
            VH16 = T(s1, "vh16", [128, 512], BF16)
            QTT = T(s1, "qtt", [128, 4, 128], BF16)
            KTT = T(s1, "ktt", [128, 4, 128], BF16)
            SCM = T(s1, "scm", [128, 4, 128], BF16)
            S32 = T(s1, "s32", [128, 4, 128], F32)
            S16 = T(s1, "s16", [128, 4, 4, 128], BF16)
            SSQ = T(s1, "ssq", [128, 4], F32)
            MH = T(s1, "mh", [128, 512], BF16)
            MHT = T(s1, "mht", [128, 512], BF16)

            for k in range(8):
                P.dma('pool', 'win', [DMA(WIN[:, k, :], w_in[k * 128:(k + 1) * 128, :])], writes=[('win', k)])
            WINR = [('win', k) for k in range(8)]
            ZT = T(s1, "zt", [128, D], BF16)
            P.op('pool', lambda e: e.memset(ZT[:], 0.0), writes=['zt'])
            xs_z = XS[0:NSLOT, :].rearrange("(n p) d -> n p d", p=128)
            for n in range(NSLOT // 128):
                P.dma('pool', 'xsz', [DMA(xs_z[n], ZT[:])], reads=['zt'], writes=['xs'])
            P.dma('sp', 'gmix', [DMA(GMIX[:], g_mix.partition_broadcast(128))], writes=['gmix'])
            P.dma('sp', 'hgg', [DMA(HGG[:], hg_gain.partition_broadcast(128))], writes=['hgg'])
            P.dma('sp', 'lb0', [DMA(QS[:], lbl[0].partition_broadcast(128))], writes=['qs'])
            P.dma('sp', 'oml', [DMA(OML[:], lbl[1].partition_broadcast(128))], writes=['oml'])
            P.op('dve', TT(OML[:], OML[:], QS[:], ALU.subtract), reads=['oml', 'qs'], writes=['oml'])
            P.op('act', ACT(OML[:], OML[:], AF.Sigmoid), reads=['oml'], writes=['oml'])
            P.op('dve', lambda e: e.memset(S32[:], 0.0), writes=['s32'])
            P.op('dve', lambda e: e.memset(S16[:], 0.0), writes=[('s16', j) for j in range(4)])

            def load_norm_T(blk, col, xb_i, pc):
                AT = ATS[pc % 2]
                xb = XB[xb_i]
                a16 = A16[xb_i]
                xk = ('xb', xb_i)
                ak = ('a16', xb_i)
                if blk == 0:
                    P.op('dve', lambda e: e.memset(xb[:], 0.0), writes=[xk])
                    P.dma('sp', xk, [DMA(xb[112:128, :], meta)], reads=[], writes=[xk])
                else:
                    P.dma('sp', xk, [DMA(xb[:], x[(blk - 1) * 128:blk * 128, :])], writes=[xk])
                P.op('act', ACT(JK[:], xb[:], AF.Square, accum_out=ST1[:, 0:1]), reads=[xk], writes=['jk', 'st1a'])
                P.op('dve', TS(ST1[:, 1:2], ST1[:, 0:1], 1.0 / D, ALU.mult, EPS, ALU.add), reads=['st1a'], writes=['st1b'])
                P.op('act', ACT(ST1[:, 2:3], ST1[:, 1:2], AF.Ln), reads=['st1b'], writes=['st1c'])
                P.op('act', ACT(ST1[:, 3:4], ST1[:, 2:3], AF.Exp, scale=-0.5), reads=['st1c'], writes=['st1d'])
                P.op('dve', STT(a16[:], xb[:], ST1[:, 3:4], GMIX[:], ALU.mult, ALU.mult), reads=[xk, 'st1d', 'gmix'], writes=[ak])
                bk, bn = bank()
                bk16 = bk[:].bitcast(BF16)
                P.op('pe', seq([TR(bk16[:, k * 128:(k + 1) * 128], a16[:, k * 128:(k + 1) * 128], ID16) for k in range(8)]),
                     reads=[ak, 'c16'], writes=[bn])
                P.op('dve', CP(AT[:, :, col * 128:(col + 1) * 128], bk16[:].rearrange("p (k t) -> p k t", k=8)),
                     reads=[bn], writes=[('at', pc % 2, col)])

            def hgrn_block2(blk, col, pc):
                AT = ATS[pc % 2]
                atr = [('at', pc % 2, col)]
                asl = lambda k: AT[:, k, col * 128:(col + 1) * 128]

                def proj(c0):
                    bk, bn = bank()
                    P.op('pe', seq([MM(bk[:], asl(k), WIN[:, k, c0:c0 + 512], start=(k == 0), stop=(k == 7)) for k in range(8)]),
                         reads=atr + WINR, writes=[bn])
                    return bk, bn
                bk, bn = proj(1024)
                P.op('act', ACT(V[:, blk, :], bk[:], AF.Copy), reads=[bn], writes=[('v', blk)])
                bk, bn = proj(1536)
                P.op('act', ACT(QS[:], bk[:], AF.Sigmoid), reads=[bn], writes=['qs'])
                P.op('dve', TT(QS[:], QS[:], bk[:], ALU.mult), reads=['qs', bn], writes=['qs'])
                bk, bn = proj(2048)
                P.op('act', ACT(SG[:], bk[:], AF.Sigmoid, scale=-1.0), reads=[bn], writes=['sg'])
                bk, bn = proj(2560)
                P.op('act', ACT(VH16[:], bk[:], AF.Copy), reads=[bn], writes=['vh16'])
                bk, bn = proj(3072)
                P.op('act', ACT(GG[:], bk[:], AF.Sigmoid), reads=[bn], writes=['gg'])
                P.op('dve', TT(GG[:], GG[:], bk[:], ALU.mult), reads=['gg', bn], writes=['gg'])
                P.op('dve', TT(GG[:], GG[:], HGG[:], ALU.mult), reads=['gg', 'hgg'], writes=['gg'])
                P.op('dve', TT(KU[:], SG[:], OML[:], ALU.mult), reads=['sg', 'oml'], writes=['ku'])
                P.op('act', ACT(LF[:], KU[:], AF.Ln, scale=-1.0, bias=1.0), reads=['ku'], writes=['lf'])
                if blk == 0:
                    P.op('dve', TS(KU[:], KU[:], RM32, ALU.mult), reads=['ku', 'c32', 'lf'], writes=['ku'])
                km, kmk = KU, 'ku'
                bbc, bbcn = bank()
                P.op('pe', MM(bbc[:], TB32, LF[:]), reads=['lf', 'c32'], writes=[bbcn])
                brc, brcn = bank()
                P.op('pe', MM(brc[:], TR32, LF[:]), reads=['lf', 'c32'], writes=[brcn])
                bdl, bdln = bank()
                P.op('pe', seq([MM(bdl[:, h * 4:(h + 1) * 4], LF[:, h * 128:(h + 1) * 128], SEL32) for h in range(4)]),
                     reads=['lf', 'c32'], writes=[bdln])
                P.op('act', ACT(EB[:], bbc[:], AF.Exp), reads=[bbcn], writes=['eb'])
                P.op('act', ACT(EMB[:], bbc[:], AF.Exp, scale=-1.0), reads=[bbcn], writes=['emb'])
                P.op('act', ACT(ERC[:], brc[:], AF.Exp), reads=[brcn], writes=['erc'])
                P.op('act', ACT(DEC[:], bdl[:, 0:16], AF.Exp), reads=[bdln], writes=['dec'])
                P.op('dve', TT(QT16[:], QS[:], EB[:], ALU.mult), reads=['qs', 'eb'], writes=['qt16'])
                P.op('dve', TT(KT16[:], km[:], EMB[:], ALU.mult), reads=[kmk, 'emb'], writes=['kt16'])
                P.op('dve', TT(KH16[:], km[:], ERC[:], ALU.mult), reads=[kmk, 'erc'], writes=['kh16'])
                if blk > 0:
                    bq, bqn = bank()
                    bq16 = bq[:].bitcast(BF16)
                    P.op('pe', seq([TR(bq16[:, h * 128:(h + 1) * 128], QT16[:, h * 128:(h + 1) * 128], ID16) for h in range(4)] +
                                   [TR(bq16[:, 512 + h * 128:512 + (h + 1) * 128], KT16[:, h * 128:(h + 1) * 128], ID16) for h in range(4)]),
                         reads=['qt16', 'kt16', 'c16'], writes=[bqn])
                    P.op('dve', CP(QTT[:].rearrange("p h t -> p (h t)"), bq16[:, 0:512]), reads=[bqn], writes=['qtt'])
                    P.op('dve', CP(KTT[:].rearrange("p h t -> p (h t)"), bq16[:, 512:1024]), reads=[bqn], writes=['ktt'])
                    bs, bsn = bank()
                    P.op('pe', seq([MM(bs[:, h * 128:(h + 1) * 128], KTT[:, h, :], QTT[:, h, :]) for h in range(4)]),
                         reads=['qtt', 'ktt'], writes=[bsn])
                    P.op('dve', TT(SCM[:], bs[:].rearrange("p (h t) -> p h t", h=4),
                                   TB32.unsqueeze(1).to_broadcast([128, 4, 128]), ALU.mult), reads=[bsn, 'c32'], writes=['scm'])
                    bo, bon = pb[blk % 2], 'pb%d' % (blk % 2)
                    P.op('pe', seq([MM(bo[:, h * 128:(h + 1) * 128], SCM[:, h, :], VH16[:, h * 128:(h + 1) * 128], start=(h == 0), stop=False)
                                    for h in range(4)]), reads=['scm', 'vh16'], writes=[bon])
                dec3 = DEC[:].rearrange("p (h j) -> p h j", h=4)
                for j in range(4):
                    if blk > 0:
                        P.op('pe', seq([MM(bo[32 * j:32 * j + 32, h * 128:(h + 1) * 128], QTT[:, h, 32 * j:32 * j + 32], S16[:, j, h, :],
                                           start=False, stop=(j == 3), tp=(0, 32 * j)) for h in range(4)]),
                             reads=['qtt', ('s16', j)], writes=[bon])
                    bu, bun = bank()
                    P.op('pe', seq([MM(bu[:, h * 128:(h + 1) * 128], KH16[32 * j:32 * j + 32, h * 128:(h + 1) * 128],
                                       VH16[32 * j:32 * j + 32, h * 128:(h + 1) * 128], tp=(32 * j, 0)) for h in range(4)]),
                         reads=['kh16', 'vh16'], writes=[bun])
                    P.op('dve', TT(S32[:], S32[:], dec3[:, :, j:j + 1].to_broadcast([128, 4, 128]), ALU.mult),
                         reads=['s32', 'dec'], writes=['s32'])
                    P.op('dve', TT(S32[:].rearrange("p h v -> p (h v)"), S32[:].rearrange("p h v -> p (h v)"), bu[:], ALU.add),
                         reads=['s32', bun], writes=['s32'])
                    jn = (j + 1) % 4
                    P.op('dve', CP(S16[:, jn, :, :], S32[:]), reads=['s32'], writes=[('s16', jn)])
                if blk == 0:
                    return
                P.op('act', ACT(EB[:], bo[:], AF.Square), reads=[bon], writes=['eb'])
                P.op('dve', lambda e: e.tensor_reduce(out=SSQ[:], in_=EB[:].rearrange("p (h v) -> p h v", h=4), axis=AX.X, op=ALU.add),
                     reads=['eb'], writes=['ssq'])
                P.op('dve', TS(SSQ[:], SSQ[:], 1.0 / 128, ALU.mult, EPS, ALU.add), reads=['ssq'], writes=['ssq'])
                P.op('act', ACT(SSQ[:], SSQ[:], AF.Ln), reads=['ssq'], writes=['ssq'])
                P.op('act', ACT(SSQ[:], SSQ[:], AF.Exp, scale=-0.5), reads=['ssq'], writes=['ssq'])
                for h in range(4):
                    P.op('dve', STT(MH[:, h * 128:(h + 1) * 128], bo[:, h * 128:(h + 1) * 128], SSQ[:, h:h + 1],
                                    GG[:, h * 128:(h + 1) * 128], ALU.mult, ALU.mult), reads=[bon, 'ssq', 'gg'], writes=[('mh', h)])
                bt, btn = bank()
                bt16 = bt[:].bitcast(BF16)
                P.op('pe', seq([TR(bt16[:, h * 128:(h + 1) * 128], MH[:, h * 128:(h + 1) * 128], ID16) for h in range(4)]),
                     reads=[('mh', h) for h in range(4)] + ['c16'], writes=[btn])
                P.op('dve', CP(MHT[:], bt16[:, 0:512]), reads=[btn], writes=['mht'])
                P.dma('sp', 'mhs', [DMA(MHS[blk], MHT[:])], reads=['mht'], writes=[('mhs', blk)])

            def front1(pc):
                blks = [0] if pc == 0 else list(range(4 * (pc - 1) + 1, 4 * pc + 1))
                AT = ATS[pc % 2]
                nt = 128 * len(blks)
                for c, blk in enumerate(blks):
                    load_norm_T(blk, c, c % 2, pc)
                atr = [('at', pc % 2, c) for c in range(len(blks))]
                for et in range(8):
                    if pc == 0 and et < 4:
                        continue
                    bk, bn = bank()
                    P.op('pe', seq([MM(bk[:, 0:nt], WIN[:, k, et * 128:(et + 1) * 128], AT[:, k, 0:nt], start=(k == 0), stop=(k == 7))
                                    for k in range(8)]), reads=atr + WINR, writes=[bn])
                    if et < 4:
                        P.op('act', ACT(QTP[:, et, :], bk[:], AF.Copy), reads=[bn], writes=[('qtp', et)])
                    else:
                        t0 = blks[0] * 128
                        P.op('act', ACT(KT[:, et - 4, t0:t0 + nt], bk[:, 0:nt], AF.Copy), reads=[bn], writes=[('kt', pc)])
                if pc >= 1:
                    P.dma('sp', 'qts', [DMA(QTS[pc - 1], QTP[:].rearrange("p e t -> p (e t)"))],
                          reads=[('qtp', et) for et in range(4)], writes=[('qts', pc)])

            def back1(pc):
                blks = [0] if pc == 0 else list(range(4 * (pc - 1) + 1, 4 * pc + 1))
                for c, blk in enumerate(blks):
                    hgrn_block2(blk, c, pc)

            front1(0)
            for pc in range(NPC + 1):
                run_with_side(lambda pc=pc: back1(pc), (lambda pc=pc: front1(pc + 1)) if pc < NPC else None, 4)
            P.barrier()
            P.emit()

        with ExitStack() as s2:
            BANKS[0] = [4, 5]
            WOUT = T(s2, "wout", [128, 8, D], BF16)
            WR = T(s2, "wr", [128, 8, 36], BF16)
            BR = T(s2, "br", [128, 36], F32)
            SBG = T(s2, "sbg", [128, 512], F32)
            GFFN = T(s2, "gffn", [128, D], F32)
            QTB = [T(s2, "qt%d" % i, [128, 4, 512], BF16) for i in range(2)]
            QTNB = [T(s2, "qtn%d" % i, [128, 4, 512], BF16) for i in range(2)]
            E32 = [T(s2, "e32%d" % i, [128, 2, 512], F32) for i in range(2)]
            SP16 = [T(s2, "sp16%d" % i, [128, 2, 512], BF16) for i in range(4)]
            W16 = [T(s2, "w16%d" % i, [128, 2, 512], BF16) for i in range(3)]
            SACC = [T(s2, "sacc%d" % i, [128, 2, 512], BF16) for i in range(2)]
            OSB = [T(s2, "osb%d" % i, [128, 4, 512], F32) for i in range(2)]
            SQ = T(s2, "sq", [128, 512], F32)
            ST2 = T(s2, "st2", [128, 8], F32)
            ST3 = T(s2, "st3", [128, 8], F32)
            MX = T(s2, "mx", [128, 512], F32)
            MX16 = T(s2, "mx16", [128, 512], BF16)
            MXT = T(s2, "mxt", [128, 8, 128], BF16)
            XR = T(s2, "xr", [128, D], F32)
            H1 = T(s2, "h1", [128, D], F32)
            JK2 = T(s2, "jk2", [128, D], F32)
            M16 = [T(s2, "m16%d" % i, [128, D], BF16) for i in range(2)]
            MT = T(s2, "mt", [128, 8, 128], BF16)
            LG = T(s2, "lg", [128, 36], F32)
            RT = T(s2, "rt", [128, 16], F32)
            GE = T(s2, "ge", [128, 4], F32)
            GSEL = T(s2, "gsel", [128, 4], F32)
            ML = T(s2, "ml", [128, 32], F32)
            ML2 = T(s2, "ml2", [128, 32], F32)
            M1 = T(s2, "m1", [128, 32], F32)
            M2 = T(s2, "m2", [128, 32], F32)
            M12 = T(s2, "m12", [128, 32], BF16)
            CNT = T(s2, "cnt", [128, 32], F32)
            DST = T(s2, "dst", [128, 32], F32)
            DJ = T(s2, "dj", [128, 32], F32)

            def load_qt(pc):
                P.dma('sp', ('qt', pc % 2), [DMA(QTB[pc % 2][:].rearrange("p e t -> p (e t)"), QTS[pc - 1])],
                      reads=[('qts', pc)], writes=[('qt', pc % 2)])
            for pc_ in range(1, min(NPC, 2) + 1):
                load_qt(pc_)
            conv = [f for ex in range(NE) for f in ((WGS, w_eg, ex), (WUS, w_eu, ex), (WDS, w_ed, ex))]
            conv_i = [0]
            stepc = [0]

            def conv_next():
                if conv_i[0] < len(conv):
                    dst, src, ex = conv[conv_i[0]]
                    conv_i[0] += 1
                    P.dma('pool', 'wcv', [DMA(dst[ex], src[ex])], writes=[('wcv', ex, conv_i[0] % 3)])
            for k in range(8):
                P.dma('pool', 'wout', [DMA(WOUT[:, k, :], w_out[k * 128:(k + 1) * 128, :])], writes=[('wout', k)])
            P.dma('pool', 'wr', [DMA(WR[:], w_rt.rearrange("(k p) e -> p k e", p=128))], writes=['wr'])
            P.dma('sp', 'br', [DMA(BR[:], b_rt.partition_broadcast(128))], writes=['br'])
            P.dma('sp', 'sbg', [DMA(SBG[:], sb_gain.partition_broadcast(128))], writes=['sbg'])
            P.dma('sp', 'gffn', [DMA(GFFN[:], g_ffn.partition_broadcast(128))], writes=['gffn'])
            P.op('dve', lambda e: e.memset(CNT[:], 0.0), writes=['cnt'])
            WOUTR = [('wout', k) for k in range(8)]
            tcount = [0]

            def sb_piece(pc, sides=()):
                b0 = 4 * (pc - 1) + 1
                osb = OSB[pc % 2]
                QT, QTN = QTB[pc % 2], QTNB[pc % 2]
                qk, qnk = ('qt', pc % 2), ('qtn', pc % 2)
                if 2 <= pc < NPC:
                    load_qt(pc + 1)
                P.op('dve', TS(QTN[:], QT[:], -0.125, ALU.mult), reads=[qk], writes=[qnk])
                kb_first = b0 + 3
                zsel = [0]
                for g in range(2):
                    for i in range(2):
                        P.op('dve', lambda e, i=i: e.memset(SACC[i][:], 0.0), writes=[('sacc', i)])
                    for bi in range(2):
                        P.op('dve', lambda e, bi=bi: e.memset(pb[bi][:], 0.0), writes=['pb%d' % bi])
                    tiles = []
                    for kb in range(kb_first, -1, -1):
                        for i in range(2):
                            t = 2 * g + i
                            j = kb - b0
                            c0 = 128 * j if j > 0 else 0
                            tiles.append(dict(i=i, t=t, kb=kb, j=j, c0=c0, n=512 - c0))
                    nt = len(tiles)

                    zkeys = ['pb2', 'pb3']
                    z2 = PALL[:, 2 * 512:4 * 512].rearrange("p (u c) -> p u c", u=2)

                    def stageA_pe(T_):
                        n, c0, t, kb = T_['n'], T_['c0'], T_['t'], T_['kb']
                        P.op('pe', seq([MM(z2[:, u, 0:n], KT[64 * u:64 * u + 64, t, kb * 128:(kb + 1) * 128],
                                           QT[64 * u:64 * u + 64, t, c0:512], tp=(64 * u, 0)) for u in range(2)]),
                             reads=[qk, ('kt', (kb + 3) // 4)], writes=zkeys)

                    def stageA(T_):
                        k = tcount[0]
                        tcount[0] += 1
                        T_['e'], T_['s'], T_['w'] = k % 2, k % 4, k % 3
                        n, c0, t, kb = T_['n'], T_['c0'], T_['t'], T_['kb']
                        e32, sp16 = E32[T_['e']], SP16[T_['s']]
                        ek, sk = ('e32', T_['e']), ('sp16', T_['s'])
                        P.op('act', ACT(e32[:, :, 0:n], z2[:, :, 0:n], AF.Exp, scale=0.125), reads=zkeys, writes=[ek])
                        P.op('act', ACT(sp16[:, :, 0:n], e32[:, :, 0:n], AF.Ln, bias=1.0), reads=[ek], writes=[sk])
                        if T_['j'] >= 0:
                            P.op('dve', TT(sp16[:, :, 0:128], sp16[:, :, 0:128], MD16.unsqueeze(1).to_broadcast([128, 2, 128]), ALU.mult),
                                 reads=[sk, 'c16'], writes=[sk])
                        if kb == 0:
                            P.op('dve', TS(sp16[:, :, 0:n], sp16[:, :, 0:n], RM32, ALU.mult), reads=[sk, 'c32'], writes=[sk])

                    def stageB(T_):
                        n, c0, t, kb, i = T_['n'], T_['c0'], T_['t'], T_['kb'], T_['i']
                        sp16, w16 = SP16[T_['s']], W16[T_['w']]
                        sk, wk, sak = ('sp16', T_['s']), ('w16', T_['w']), ('sacc', i)
                        t2 = PALL[:, 6 * 512:8 * 512].rearrange("p (u c) -> p u c", u=2)
                        mms = []
                        for u in range(2):
                            mms.append(MM(t2[:, u, 0:n], UP16, sp16[:, u, 0:n], start=True, stop=False))
                            if kb != kb_first:
                                mms.append(MM(t2[:, u, 0:n], ONE16, SACC[i][:, u, c0:512], start=False, stop=False))
                        for u in range(2):
                            kts = KT[64 * u:64 * u + 64, t, kb * 128:(kb + 1) * 128]
                            mms.append(MM(t2[:, u, 0:n], kts, QTN[64 * u:64 * u + 64, t, c0:512], start=False, stop=True, tp=(64 * u, 0)))
                        P.op('pe', seq(mms), reads=[sk, sak, qnk, 'c16', ('kt', (kb + 3) // 4)], writes=['pb6', 'pb7'])
                        P.op('act', ACT(w16[:, :, 0:n], t2[:, :, 0:n], AF.Exp, scale=-1.0), reads=['pb6', 'pb7'], writes=[wk])
                        if T_['j'] >= 0:
                            P.op('dve', TT(w16[:, :, 0:128], w16[:, :, 0:128], MD16.unsqueeze(1).to_broadcast([128, 2, 128]), ALU.mult),
                                 reads=[wk, 'c16'], writes=[wk])
                        if kb == 0:
                            P.op('dve', TS(w16[:, :, 0:n], w16[:, :, 0:n], RM32, ALU.mult), reads=[wk, 'c32'], writes=[wk])

                    def stageC(T_):
                        n, c0, kb, i, t, j = T_['n'], T_['c0'], T_['kb'], T_['i'], T_['t'], T_['j']
                        sp16, w16 = SP16[T_['s']], W16[T_['w']]
                        sk, wk, sak = ('sp16', T_['s']), ('w16', T_['w']), ('sacc', i)
                        if kb > 0:
                            P.op('dve', TT(SACC[i][:, :, c0:512], SACC[i][:, :, c0:512], sp16[:, :, 0:n], ALU.add), reads=[sak, sk], writes=[sak])
                        q0 = j if j > 0 else 0
                        ob, obn = pb[i], 'pb%d' % i
                        mms = []
                        for u in range(2):
                            h = 2 * t + u
                            for q in range(q0, 4):
                                mms.append(MM(ob[:, u * 256 + q * 64:u * 256 + (q + 1) * 64], w16[:, u, (q - q0) * 128:(q - q0 + 1) * 128],
                                              V[:, kb, h * 64:(h + 1) * 64], start=False, stop=(kb == 0)))
                        P.op('pe', seq(mms), reads=[wk, ('v', kb)], writes=[obn])

                    stageA_pe(tiles[0])
                    for step in range(nt + 2):
                        if step < nt:
                            stageA(tiles[step])
                        if 0 <= step - 1 < nt:
                            stageB(tiles[step - 1])
                        if 0 <= step - 2 < nt:
                            stageC(tiles[step - 2])
                        if step + 1 < nt:
                            stageA_pe(tiles[step + 1])
                        stepc[0] += 1
                        if stepc[0] % CONV_EVERY == 0:
                            conv_next()
                        for sd in sides:
                            sd.step(6)
                    for i in range(2):
                        t = 2 * g + i
                        P.op('dve', CP(osb[:, :, t * 128:(t + 1) * 128].rearrange("p q (u d) -> p q u d", u=2),
                                       pb[i][:, :].rearrange("p (u q d) -> p q u d", u=2, q=4)),
                             reads=['pb%d' % i], writes=[('osb', pc % 2, 2 * t), ('osb', pc % 2, 2 * t + 1)])

            def rstd_chain(buf, n, scale, kbase):
                P.op('dve', TS(buf[:, 0:n], buf[:, 0:n], scale, ALU.mult, EPS, ALU.add), reads=[kbase], writes=[kbase])
                P.op('act', ACT(buf[:, 0:n], buf[:, 0:n], AF.Ln), reads=[kbase], writes=[kbase])
                P.op('act', ACT(buf[:, 0:n], buf[:, 0:n], AF.Exp, scale=-0.5), reads=[kbase], writes=[kbase])

            def post_block(pc, q):
                blk = 4 * (pc - 1) + 1 + q
                osr = [('osb', pc % 2, h) for h in range(8)]
                oq = OSB[pc % 2][:, q, :]
                P.dma('sp', 'mxtb', [DMA(MXT[:, 4:8, :].rearrange("p k t -> p (k t)"), MHS[blk])], reads=[('mhs', blk)], writes=['mxtb'])
                P.dma('sp', 'xr', [DMA(XR[:], x[(blk - 1) * 128:blk * 128, :])], writes=['xr'])
                P.op('dve', TT(SQ[:], oq, oq, ALU.mult), reads=osr, writes=['sq'])
                P.op('dve', lambda e: e.tensor_reduce(out=ST2[:], in_=SQ[:].rearrange("p (h d) -> p h d", h=8), axis=AX.X, op=ALU.add),
                     reads=['sq'], writes=['st2'])
                rstd_chain(ST2, 8, 1.0 / 64, 'st2')
                P.op('dve', TT(MX[:].rearrange("p (h d) -> p h d", h=8), oq.rearrange("p (h d) -> p h d", h=8),
                               ST2[:].unsqueeze(2).to_broadcast([128, 8, 64]), ALU.mult), reads=osr + ['st2'], writes=['mx'])
                P.op('dve', TT(MX16[:], MX[:], SBG[:], ALU.mult), reads=['mx', 'sbg'], writes=['mx16'])
                bt, btn = bank()
                bt16 = bt[:].bitcast(BF16)
                P.op('pe', seq([TR(bt16[:, k * 128:(k + 1) * 128], MX16[:, k * 128:(k + 1) * 128], ID16) for k in range(4)]),
                     reads=['mx16', 'c16'], writes=[btn])
                P.op('dve', CP(MXT[:, 0:4, :].rearrange("p k t -> p (k t)"), bt16[:, 0:512]), reads=[btn], writes=['mxta'])
                for hf in range(2):
                    bo, bon = bank()
                    P.op('pe', seq([MM(bo[:], MXT[:, k, :], WOUT[:, k, hf * 512:(hf + 1) * 512], start=(k == 0), stop=(k == 7)) for k in range(8)]),
                         reads=['mxta', 'mxtb'] + WOUTR, writes=[bon])
                    P.op('dve', TT(H1[:, hf * 512:(hf + 1) * 512], bo[:], XR[:, hf * 512:(hf + 1) * 512], ALU.add),
                         reads=[bon, 'xr'], writes=[('h1', hf)])
                h1r = [('h1', 0), ('h1', 1)]
                P.dma('sp', 'h1s', [DMA(H1S[blk], H1[:])], reads=h1r, writes=[('h1s', blk)])
                P.op('act', ACT(JK2[:], H1[:], AF.Square, accum_out=ST3[:, 0:1]), reads=h1r, writes=['jk2', 'st3'])
                rstd_chain(ST3, 1, 1.0 / D, 'st3')
                mi = blk % 2
                m16, mk = M16[mi], ('m16', mi)
                P.op('dve', STT(m16[:], H1[:], ST3[:, 0:1], GFFN[:], ALU.mult, ALU.mult), reads=h1r + ['st3', 'gffn'], writes=[mk])
                bt, btn = bank()
                bt16 = bt[:].bitcast(BF16)
                P.op('pe', seq([TR(bt16[:, k * 128:(k + 1) * 128], m16[:, k * 128:(k + 1) * 128], ID16) for k in range(8)]),
                     reads=[mk, 'c16'], writes=[btn])
                P.op('dve', CP(MT[:].rearrange("p k t -> p (k t)"), bt16[:]), reads=[btn], writes=['mt'])
                bl, bln = bank()
                P.op('pe', seq([MM(bl[:, 0:36], MT[:, k, :], WR[:, k, :], start=(k == 0), stop=(k == 7)) for k in range(8)]),
                     reads=['mt', 'wr'], writes=[bln])
                P.op('dve', TT(LG[:], bl[:, 0:36], BR[:], ALU.add), reads=[bln, 'br'], writes=['lg'])
                r = lambda i: RT[:, i:i + 1]
                P.op('dve', lambda e: e.reduce_max(out=r(0), in_=LG[:, 0:4], axis=AX.X), reads=['lg'], writes=['r0'])
                P.op('dve', TS(r(1), r(0), -1.0, ALU.mult), reads=['r0'], writes=['r1'])
                P.op('act', ACT(GE[:], LG[:, 0:4], AF.Exp, bias=r(1), accum_out=r(2)), reads=['lg', 'r1'], writes=['ge', 'r2'])
                P.op('dve', lambda e: e.reciprocal(out=r(3), in_=r(2)), reads=['r2'], writes=['r3'])
                P.op('dve', TS(GSEL[:], LG[:, 0:4], r(0), ALU.is_equal), reads=['lg', 'r0'], writes=['gsel'])
                P.op('dve', TS(GSEL[:], GSEL[:], 1.0, ALU.subtract, BIG, ALU.mult), reads=['gsel'], writes=['gsel'])
                P.op('dve', TT(ML[:].rearrange("p (g j) -> p g j", g=4), LG[:, 4:36].rearrange("p (g j) -> p g j", g=4),
                               GSEL[:].unsqueeze(2).to_broadcast([128, 4, 8]), ALU.add), reads=['lg', 'gsel'], writes=['ml'])
                P.op('dve', lambda e: e.reduce_max(out=r(4), in_=ML[:], axis=AX.X), reads=['ml'], writes=['r4'])
                P.op('dve', TS(M1[:], ML[:], r(4), ALU.is_equal), reads=['ml', 'r4'], writes=['m1'])
                P.op('dve', STT(ML2[:], M1[:], -BIG, ML[:], ALU.mult, ALU.add), reads=['m1', 'ml'], writes=['ml2'])
                P.op('dve', lambda e: e.reduce_max(out=r(5), in_=ML2[:], axis=AX.X), reads=['ml2'], writes=['r5'])
                P.op('dve', TS(M2[:], ML2[:], r(5), ALU.is_equal), reads=['ml2', 'r5'], writes=['m2'])
                P.op('dve', TT(r(6), r(5), r(4), ALU.subtract), reads=['r4', 'r5'], writes=['r6'])
                P.op('act', ACT(r(7), r(6), AF.Exp), reads=['r6'], writes=['r7'])
                P.op('dve', TS(r(8), r(7), 1.0, ALU.add), reads=['r7'], writes=['r8'])
                P.op('dve', lambda e: e.reciprocal(out=r(9), in_=r(8)), reads=['r8'], writes=['r9'])
                P.op('dve', TT(G1[:, blk:blk + 1], r(9), r(3), ALU.mult), reads=['r9', 'r3'], writes=[('g1', blk)])
                P.op('dve', TT(G2[:, blk:blk + 1], r(3), G1[:, blk:blk + 1], ALU.subtract), reads=['r3', ('g1', blk)], writes=[('g2', blk)])
                P.op('dve', TT(M12[:], M1[:], M2[:], ALU.add), reads=['m1', 'm2'], writes=['m12'])
                bc_, bcn = bank()
                P.op('pe', seq([MM(bc_[:, 0:32], MD16, M12[:]), MM(bc_[:, 32:64], ONE16, M12[:])]), reads=['m12', 'c16'], writes=[bcn])
                P.op('dve', TT(DST[:], bc_[:, 0:32], CNT[:], ALU.add), reads=[bcn, 'cnt'], writes=['dst'])
                P.op('dve', TT(CNT[:], CNT[:], bc_[:, 32:64], ALU.add), reads=[bcn, 'cnt', 'dst'], writes=['cnt'])
                P.op('dve', TS(DST[:], DST[:], float(CAP - 1), ALU.min), reads=['dst'], writes=['dst'])
                P.op('dve', TT(DST[:], DST[:], OFF32, ALU.add), reads=['dst', 'c32'], writes=['dst'])
                for (mm_, ds, nm) in ((M1, DS1, 'ds1'), (M2, DS2, 'ds2')):
                    P.op('dve', TT(DJ[:], DST[:], mm_[:], ALU.mult), reads=['dst', 'm1', 'm2'], writes=['dj'])
                    P.op('dve', lambda e: e.tensor_reduce(out=r(10), in_=DJ[:], axis=AX.X, op=ALU.add), reads=['dj'], writes=['r10'])
                    P.op('dve', CP(ds[:, blk:blk + 1], r(10)), reads=['r10'], writes=[(nm, blk)])
                    P.dma('pool', ('xs', nm), [lambda e, ds=ds, m16=m16: e.indirect_dma_start(
                        out=XS, out_offset=bass.IndirectOffsetOnAxis(ap=ds[:, blk:blk + 1], axis=0), in_=m16[:], in_offset=None,
                        bounds_check=BCR, oob_is_err=False)], reads=[(nm, blk), mk, 'xs'], writes=[('xsw', nm, blk)])

            def post_piece(pc):
                for q in range(4):
                    post_block(pc, q)

            for pc in range(1, NPC + 1):
                sides = [Side(lambda pc=pc: post_piece(pc - 1))] if pc > 1 else []
                sb_piece(pc, sides)
                for sd in sides:
                    sd.drain()
            while conv_i[0] < len(conv):
                conv_next()
            post_piece(NPC)
            P.barrier()
            P.emit()
        s12.close()

        with ExitStack() as s3:
            BANKS[0] = [2, 3, 4, 5, 6, 7]
            WG = [T(s3, "wg%d" % i, [128, 8, 512], BF16) for i in range(2)]
            WU = [T(s3, "wu%d" % i, [128, 8, 512], BF16) for i in range(2)]
            WD = [T(s3, "wd%d" % i, [128, 4, D], BF16) for i in range(2)]
            XG = [T(s3, "xg%d" % i, [128, 3, D], BF16) for i in range(2)]
            XTS = [T(s3, "xt%d" % i, [128, 8, CAP], BF16) for i in range(2)]
            SL = T(s3, "sl", [128, CAP], F32)
            HB = T(s3, "hb", [128, 4, CAP], BF16)
            YT = [T(s3, "yt%d" % i, [128, D], BF16) for i in range(2)]
            GFIN = T(s3, "gfin", [128, D], F32)
            Y1 = [T(s3, "y1%d" % i, [128, D], BF16) for i in range(4)]
            Y2 = [T(s3, "y2%d" % i, [128, D], BF16) for i in range(4)]
            HF = [T(s3, "hf%d" % i, [128, D], F32) for i in range(4)]
            JK3 = T(s3, "jk3", [128, D], F32)
            ST4 = T(s3, "st4", [128, 4], F32)
            P.dma('sp', 'gfin', [DMA(GFIN[:], g_fin.partition_broadcast(128))], writes=['gfin'])
            ycount = [0]

            def front3(ex):
                i2 = ex % 2
                wg, wu, wd, xg, xt = WG[i2], WU[i2], WD[i2], XG[i2], XTS[i2]
                P.dma('sp', ('xg', i2), [DMA(xg[:], XS[ex * CAP:(ex + 1) * CAP, :].rearrange("(s p) d -> p s d", p=128))], writes=[('xg', i2)])
                P.dma('sp', ('wg', i2), [DMA(wg[:], WGS[ex].rearrange("(k p) f -> p k f", p=128))], reads=[('wcv', ex, c_) for c_ in range(3)], writes=[('wg', i2)])
                P.dma('sp', ('wu', i2), [DMA(wu[:], WUS[ex].rearrange("(k p) f -> p k f", p=128))], reads=[('wcv', ex, c_) for c_ in range(3)], writes=[('wu', i2)])
                P.dma('pool', ('wd', i2), [DMA(wd[:], WDS[ex].rearrange("(k p) f -> p k f", p=128))], reads=[('wcv', ex, c_) for c_ in range(3)], writes=[('wd', i2)])
                for s_ in range(3):
                    bt, btn = bank()
                    bt16 = bt[:].bitcast(BF16)
                    P.op('pe', seq([TR(bt16[:, k * 128:(k + 1) * 128], xg[:, s_, k * 128:(k + 1) * 128], ID16) for k in range(8)]),
                         reads=[('xg', i2), 'c16'], writes=[btn])
                    src = bt16[:].rearrange("p (k t) -> p k t", k=8)
                    dst = xt[:, :, s_ * 128:(s_ + 1) * 128]
                    if s_ == 1:
                        P.op('act', (lambda e, dst=dst, src=src: e.copy(out=dst, in_=src)), reads=[btn], writes=[('xt', i2, s_)])
                    else:
                        P.op('dve', CP(dst, src), reads=[btn], writes=[('xt', i2, s_)])

            def back3(ex):
                i2 = ex % 2
                wg, wu, wd, xt = WG[i2], WU[i2], WD[i2], XTS[i2]
                xtr = [('xt', i2, s_) for s_ in range(3)]
                for ft in range(4):
                    bg, bgn = bank()
                    P.op('pe', seq([MM(bg[:, 0:CAP], wg[:, k, ft * 128:(ft + 1) * 128], xt[:, k, :], start=(k == 0), stop=(k == 7)) for k in range(8)]),
                         reads=xtr + [('wg', i2)], writes=[bgn])
                    bu, bun = bank()
                    P.op('pe', seq([MM(bu[:, 0:CAP], wu[:, k, ft * 128:(ft + 1) * 128], xt[:, k, :], start=(k == 0), stop=(k == 7)) for k in range(8)]),
                         reads=xtr + [('wu', i2)], writes=[bun])
                    P.op('act', ACT(SL[:], bg[:, 0:CAP], AF.Silu), reads=[bgn], writes=['sl'])
                    P.op('dve', TT(HB[:, ft, :], SL[:], bu[:, 0:CAP], ALU.mult), reads=['sl', bun], writes=[('hb', ft)])
                hbr = [('hb', ft) for ft in range(4)]
                for s_ in range(3):
                    yi = ycount[0] % 2
                    ycount[0] += 1
                    yt, yk = YT[yi], ('yt', yi)
                    for hf in range(2):
                        by, byn = bank()
                        P.op('pe', seq([MM(by[:], HB[:, ft, s_ * 128:(s_ + 1) * 128], wd[:, ft, hf * 512:(hf + 1) * 512], start=(ft == 0), stop=(ft == 3))
                                        for ft in range(4)]), reads=hbr + [('wd', i2)], writes=[byn])
                        if hf == 0:
                            P.op('act', ACT(yt[:, 0:512], by[:], AF.Copy), reads=[byn], writes=[(yk, 0)])
                        else:
                            P.op('dve', CP(yt[:, 512:1024], by[:]), reads=[byn], writes=[(yk, 1)])
                    r0 = ex * CAP + s_ * 128
                    P.dma('sp', ('ys', yi), [DMA(YS[r0:r0 + 128, :], yt[:])], reads=[(yk, 0), (yk, 1)], writes=[('ys', ex, s_)])

            front3(0)
            for ex in range(NE):
                run_with_side(lambda ex=ex: back3(ex), (lambda ex=ex: front3(ex + 1)) if ex + 1 < NE else None, 3)
            ysall = [('ys', ex, s) for ex in range(NE) for s in range(3)]
            stores = []
            for blk in range(1, NB):
                i2 = blk % 4
                y1, y2, hf_ = Y1[i2], Y2[i2], HF[i2]
                P.dma('pool', ('y1', i2), [lambda e, y1=y1, blk=blk: e.indirect_dma_start(
                    out=y1[:], out_offset=None, in_=YS, in_offset=bass.IndirectOffsetOnAxis(ap=DS1[:, blk:blk + 1], axis=0),
                    bounds_check=BCR, oob_is_err=False)], reads=ysall + [('ds1', blk)], writes=[('y1', i2)])
                P.dma('pool', ('y2', i2), [lambda e, y2=y2, blk=blk: e.indirect_dma_start(
                    out=y2[:], out_offset=None, in_=YS, in_offset=bass.IndirectOffsetOnAxis(ap=DS2[:, blk:blk + 1], axis=0),
                    bounds_check=BCR, oob_is_err=False)], reads=ysall + [('ds2', blk)], writes=[('y2', i2)])
                P.dma('sp', ('hf', i2), [DMA(hf_[:], H1S[blk])], reads=[('h1s', blk)], writes=[('hf', i2)])
                P.op('dve', STT(hf_[:], y1[:], G1[:, blk:blk + 1], hf_[:], ALU.mult, ALU.add), reads=[('y1', i2), ('g1', blk), ('hf', i2)], writes=[('hf', i2)])
                P.op('dve', STT(hf_[:], y2[:], G2[:, blk:blk + 1], hf_[:], ALU.mult, ALU.add), reads=[('y2', i2), ('g2', blk), ('hf', i2)], writes=[('hf', i2)])
                P.op('act', ACT(JK3[:], hf_[:], AF.Square, accum_out=ST4[:, 0:1]), reads=[('hf', i2)], writes=['jk3', 'st4'])
                rstd_chain(ST4, 1, 1.0 / D, 'st4')
                P.op('dve', STT(hf_[:], hf_[:], ST4[:, 0:1], GFIN[:], ALU.mult, ALU.mult), reads=[('hf', i2), 'st4', 'gfin'], writes=[('hf', i2)])
                stores.append(P.dma('sp', ('out', i2), [DMA(out[(blk - 1) * 128:blk * 128, :], hf_[:])], reads=[('hf', i2)], writes=[('out', blk)]))
            P.final_wait('sp', stores)
            P.emit()
    return nc


_CACHE = {}


def kernel(x, meta_tokens, lb_logits, g_mix, w_in, sb_gain, hg_gain, w_out, g_ffn, w_router_group, b_router_group,
           w_router_expert, b_router_expert, w_expert_gate, w_expert_up, w_expert_down, g_final, _npc=8, _cores=8, _debug=False):
    f = lambda a: np.ascontiguousarray(np.asarray(a), dtype=np.float32)
    x = f(x)
    if (_npc, _debug) not in _CACHE:
        _CACHE[(_npc, _debug)] = build(_npc, _debug)
    nc = _CACHE[(_npc, _debug)]
    shared = dict(
        meta_tokens=f(meta_tokens), lb_logits=f(lb_logits), g_mix=f(g_mix)[0], w_in=f(w_in)[0], sb_gain=f(sb_gain)[0],
        hg_gain=f(hg_gain)[0], w_out=f(w_out)[0], g_ffn=f(g_ffn)[0],
        w_rt=np.ascontiguousarray(np.concatenate([f(w_router_group)[0], f(w_router_expert)[0]], axis=1)),
        b_rt=np.ascontiguousarray(np.concatenate([f(b_router_group)[0], f(b_router_expert)[0]], axis=0)),
        w_expert_gate=f(w_expert_gate)[0], w_expert_up=f(w_expert_up)[0], w_expert_down=f(w_expert_down)[0],
        g_final=f(g_final), consts=make_consts())
    in_maps = [dict(shared, x=np.ascontiguousarray(x[c])) for c in range(_cores)]
    res = run_bass_kernel_spmd(nc, in_maps, core_ids=list(range(_cores)))
    if _debug:
        return res.results
    return np.stack([np.asarray(r["out"], dtype=np.float32) for r in res.results], axis=0)
```
